# Optimizing a Trainium2 kernel written in Bass

```python
import jax, jax.numpy as jnp
from jax import lax
import numpy as np

D_MODEL = 1024
BATCH = 2
SEQ = 8192
DEPTH = 2

N_META = 16
CONV_WIDTH = 3
N_HEADS = 16
HEAD_DIM = 64
N_KV_HEADS = 4
Q_PER_KV = N_HEADS // N_KV_HEADS
WINDOW = 128
BLOCK = 128
ROPE_THETA = 10000.0
N_GROUPS = 4
EXPERTS_PER_GROUP = 8
N_EXPERTS = N_GROUPS * EXPERTS_PER_GROUP
TOP_K = 2
D_EXPERT = 256
N_A_LAYERS = DEPTH // 2
N_B_LAYERS = DEPTH - N_A_LAYERS
NORM_EPS = 1e-5
NEG_INF = -1e30

kernel_name = "yoco_shortconv_swa_sink_hier_moe"


def rms_norm(x, g):
    xf = x.astype(jnp.float32)
    y = xf * lax.rsqrt(jnp.mean(xf * xf, axis=-1, keepdims=True) + NORM_EPS)
    return (y * g.astype(jnp.float32)).astype(x.dtype)


def rope(x, pos):
    half = HEAD_DIM // 2
    inv_freq = ROPE_THETA ** (-jnp.arange(half, dtype=jnp.float32) / half)
    ang = pos.astype(jnp.float32)[:, None] * inv_freq[None, :]
    cos = jnp.cos(ang)[None, :, None, :]
    sin = jnp.sin(ang)[None, :, None, :]
    xf = x.astype(jnp.float32)
    x1, x2 = xf[..., :half], xf[..., half:]
    out = jnp.concatenate([x1 * cos - x2 * sin, x2 * cos + x1 * sin], axis=-1)
    return out.astype(x.dtype)


def short_conv_mixer(xn, w_in, conv_w, w_out):
    gate_b, gate_c, v = jnp.split(xn @ w_in, 3, axis=-1)
    u = gate_c * v
    L = u.shape[1]
    up = jnp.pad(u, ((0, 0), (CONV_WIDTH - 1, 0), (0, 0)))
    conv = up[:, 0:L] * conv_w[0]
    for i in range(1, CONV_WIDTH):
        conv = conv + up[:, i:i + L] * conv_w[i]
    return (gate_b * conv) @ w_out


def _to_blocks(t, pad):
    widths = [(0, 0), (pad, 0)] + [(0, 0)] * (t.ndim - 2)
    t = jnp.pad(t, widths)
    return t.reshape((t.shape[0], t.shape[1] // BLOCK, BLOCK) + t.shape[2:])


def _band(tb):
    widths = [(0, 0), (1, 0)] + [(0, 0)] * (tb.ndim - 2)
    prev = jnp.pad(tb[:, :-1], widths)
    return jnp.concatenate([prev, tb], axis=2)


def shared_kv(h, kv_norm_g, w_kv, pos):
    B, L, _ = h.shape
    pad = (-L) % BLOCK
    k, v = jnp.split(rms_norm(h, kv_norm_g) @ w_kv, 2, axis=-1)
    k = rope(k.reshape(B, L, N_KV_HEADS, HEAD_DIM), pos)
    v = v.reshape(B, L, N_KV_HEADS, HEAD_DIM)
    return _band(_to_blocks(k, pad)), _band(_to_blocks(v, pad))


def sliding_window_attention(xn, w_q, w_o, sinks, k_band, v_band, pos):
    B, L, _ = xn.shape
    pad = (-L) % BLOCK
    n_blk = (L + pad) // BLOCK
    q = rope((xn @ w_q).reshape(B, L, N_HEADS, HEAD_DIM), pos)
    q = _to_blocks(q, pad).reshape(B, n_blk, BLOCK, N_KV_HEADS, Q_PER_KV, HEAD_DIM)
    s = jnp.einsum('bnqkgd,bnjkd->bnkgqj', q, k_band,
                   preferred_element_type=jnp.float32) * (HEAD_DIM ** -0.5)
    qi = jnp.arange(BLOCK)[:, None]
    kj = jnp.arange(2 * BLOCK)[None, :]
    diff = qi - kj + BLOCK
    band = (diff >= 0) & (diff < WINDOW)
    kpos = (jnp.arange(n_blk)[:, None] - 1) * BLOCK + kj - pad
    valid = band[None] & (kpos >= 0)[:, None, :]
    s = jnp.where(valid[None, :, None, None], s, NEG_INF)
    sink = jnp.broadcast_to(
        sinks.astype(jnp.float32).reshape(1, 1, N_KV_HEADS, Q_PER_KV, 1, 1),
        s.shape[:-1] + (1,))
    p = jax.nn.softmax(jnp.concatenate([s, sink], axis=-1), axis=-1)[..., :-1]
    o = jnp.einsum('bnkgqj,bnjkd->bnqkgd', p.astype(v_band.dtype), v_band)
    o = o.reshape(B, n_blk * BLOCK, N_HEADS * HEAD_DIM)[:, pad:]
    return o @ w_o


def hierarchical_moe(xn, rg_w, rg_b, re_w, re_b, w_gate, w_up, w_down):
    shape = xn.shape
    t = xn.reshape(-1, shape[-1])
    T = t.shape[0]
    g_logits = (t @ rg_w + rg_b).astype(jnp.float32)
    g_idx = jnp.argmax(g_logits, axis=-1)
    g_w = jnp.take_along_axis(jax.nn.softmax(g_logits, axis=-1), g_idx[:, None], axis=-1)
    e_logits = (t @ re_w + re_b).astype(jnp.float32).reshape(T, N_GROUPS, EXPERTS_PER_GROUP)
    e_sel = jnp.take_along_axis(e_logits, g_idx[:, None, None], axis=1)[:, 0]
    top_v, top_i = lax.top_k(e_sel, TOP_K)
    top_w = jax.nn.softmax(top_v, axis=-1) * g_w
    expert_id = g_idx[:, None] * EXPERTS_PER_GROUP + top_i
    combine = jnp.sum(jax.nn.one_hot(expert_id, N_EXPERTS, dtype=jnp.float32)
                      * top_w[..., None], axis=1).astype(t.dtype)
    out = jnp.zeros_like(t)
    for e in range(N_EXPERTS):
        hdn = jax.nn.silu(t @ w_gate[e]) * (t @ w_up[e])
        out = out + (hdn * combine[:, e:e + 1]) @ w_down[e]
    return out.reshape(shape)


def setup_inputs(seed: int = 0) -> dict:
    key = jax.random.key(seed)
    ks = jax.random.split(key, 21)
    D, F, E, G = D_MODEL, D_EXPERT, N_EXPERTS, N_GROUPS
    nrm = lambda k, s, sc: jax.random.normal(k, s, jnp.float32) * sc
    gain = lambda k, s: 1.0 + 0.02 * jax.random.normal(k, s, jnp.float32)
    return {
        "x": nrm(ks[0], (BATCH, SEQ, D), 1.0),
        "meta_tokens": nrm(ks[1], (N_META, D), 1.0),
        "conv_norm_g": gain(ks[2], (N_A_LAYERS, D)),
        "conv_w_in": nrm(ks[3], (N_A_LAYERS, D, 3 * D), D ** -0.5),
        "conv_w": nrm(ks[4], (N_A_LAYERS, CONV_WIDTH, D), CONV_WIDTH ** -0.5),
        "conv_w_out": nrm(ks[5], (N_A_LAYERS, D, D), D ** -0.5),
        "kv_norm_g": gain(ks[6], (D,)),
        "w_kv": nrm(ks[7], (D, 2 * N_KV_HEADS * HEAD_DIM), D ** -0.5),
        "attn_norm_g": gain(ks[8], (N_B_LAYERS, D)),
        "w_q": nrm(ks[9], (N_B_LAYERS, D, N_HEADS * HEAD_DIM), D ** -0.5),
        "w_o": nrm(ks[10], (N_B_LAYERS, N_HEADS * HEAD_DIM, D), (N_HEADS * HEAD_DIM) ** -0.5),
        "sinks": nrm(ks[11], (N_B_LAYERS, N_HEADS), 0.5),
        "ffn_norm_g": gain(ks[12], (DEPTH, D)),
        "router_group_w": nrm(ks[13], (DEPTH, D, G), D ** -0.5),
        "router_group_b": nrm(ks[14], (DEPTH, G), 0.01),
        "router_expert_w": nrm(ks[15], (DEPTH, D, E), D ** -0.5),
        "router_expert_b": nrm(ks[16], (DEPTH, E), 0.01),
        "w_gate": nrm(ks[17], (DEPTH, E, D, F), D ** -0.5),
        "w_up": nrm(ks[18], (DEPTH, E, D, F), D ** -0.5),
        "w_down": nrm(ks[19], (DEPTH, E, F, D), F ** -0.5),
        "final_norm_g": gain(ks[20], (D,)),
    }


def reference(x, meta_tokens, conv_norm_g, conv_w_in, conv_w, conv_w_out, kv_norm_g, w_kv,
              attn_norm_g, w_q, w_o, sinks, ffn_norm_g, router_group_w, router_group_b,
              router_expert_w, router_expert_b, w_gate, w_up, w_down, final_norm_g):
    B = x.shape[0]
    meta = jnp.broadcast_to(meta_tokens.astype(x.dtype)[None], (B, N_META, x.shape[-1]))
    h = jnp.concatenate([meta, x], axis=1)
    pos = jnp.arange(h.shape[1], dtype=jnp.int32)
    k_band, v_band = None, None
    for layer in range(DEPTH):
        if layer < N_A_LAYERS:
            i = layer
            h = h + short_conv_mixer(rms_norm(h, conv_norm_g[i]), conv_w_in[i], conv_w[i], conv_w_out[i])
        else:
            j = layer - N_A_LAYERS
            if j == 0:
                k_band, v_band = shared_kv(h, kv_norm_g, w_kv, pos)
            h = h + sliding_window_attention(rms_norm(h, attn_norm_g[j]), w_q[j], w_o[j], sinks[j],
                                             k_band, v_band, pos)
        h = h + hierarchical_moe(rms_norm(h, ffn_norm_g[layer]), router_group_w[layer],
                                 router_group_b[layer], router_expert_w[layer], router_expert_b[layer],
                                 w_gate[layer], w_up[layer], w_down[layer])
    h = rms_norm(h, final_norm_g)
    return h[:, N_META:]
```

```python
import numpy as np
from contextlib import ExitStack
import concourse.bass as bass
import concourse.mybir as mybir
from concourse.bass_utils import run_bass_kernel_spmd

F32 = mybir.dt.float32
BF16 = mybir.dt.bfloat16
ALU = mybir.AluOpType
AF = mybir.ActivationFunctionType
AX = mybir.AxisListType

D = 1024
NCH = 8
SEQ = 8192
NMETA = 16
HALO = 130
OWN = 2048
TT = HALO + OWN
NKV = TT - 2
NKB = NKV // 128
NE = 32
EPS = 1e-5
TW = 256
SAME_ENGINE_SYNC = True

L0_CONV_TILES = [(0, 130)] + [(HALO + TW * k, TW) for k in range(OWN // TW)]
L0_MOE_TILES = [(2, 128)] + [(HALO + TW * k, TW) for k in range(OWN // TW)]
L1_TILES = [(HALO + TW * k, TW) for k in range(OWN // TW)]


def tile_idx(t0):
    return 0 if t0 < HALO else 1 + (t0 - HALO) // TW


class Buf:
    __slots__ = ("name", "w", "r")

    def __init__(self, name):
        self.name = name
        self.w = None
        self.r = {}


class Tracker:
    def __init__(self, nc, es, same_engine_sync=True):
        self.nc = nc
        self.eng = {"pe": nc.tensor, "act": nc.scalar, "dve": nc.vector, "pool": nc.gpsimd, "sp": nc.sync}
        self.sem = {k: es.enter_context(nc.semaphore("s_" + k)) for k in ("pe", "act", "dve", "pool")}
        self.tick = {k: 0 for k in self.sem}
        self.waited = {}
        self.dsem = {}
        self.es = es
        self.same = same_engine_sync
        self.nwait = 0

    def _sem_of(self, prod):
        return self.sem[prod] if prod in self.sem else self.dsem[prod][0]

    def _wait(self, cons, prod, tick):
        if prod == cons and (cons == "pe" or not self.same):
            return
        key = (cons, prod)
        if self.waited.get(key, 0) >= tick:
            return
        self.waited[key] = tick
        self.eng[cons].wait_ge(self._sem_of(prod), tick)
        self.nwait += 1

    def deps(self, cons, reads, writes):
        need = {}
        for b in reads:
            if b.w is not None:
                need[b.w[0]] = max(need.get(b.w[0], 0), b.w[1])
        for b in writes:
            if b.w is not None:
                need[b.w[0]] = max(need.get(b.w[0], 0), b.w[1])
            for p, t in b.r.items():
                need[p] = max(need.get(p, 0), t)
        for p, t in need.items():
            self._wait(cons, p, t)

    def op(self, eng, fn, reads=(), writes=()):
        self.deps(eng, reads, writes)
        ins = fn(self.eng[eng])
        self.tick[eng] += 1
        t = self.tick[eng]
        ins.then_inc(self.sem[eng], 1)
        for b in reads:
            b.r[eng] = t
        for b in writes:
            b.w = (eng, t)
            b.r = {}
        return t

    def new_dsem(self, name):
        if name not in self.dsem:
            self.dsem[name] = [self.es.enter_context(self.nc.semaphore("d_" + name)), 0]

    def dma(self, queue, dsem, items):
        self.new_dsem(dsem)
        for (_, _, reads, writes) in items:
            self.deps(queue, reads, writes)
        sem = self.dsem[dsem][0]
        for (o, i, _, _) in items:
            self.eng[queue].dma_start(out=o, in_=i).then_inc(sem, 16)
            self.dsem[dsem][1] += 16
        cnt = self.dsem[dsem][1]
        for (_, _, reads, writes) in items:
            for b in reads:
                b.r[dsem] = cnt
            for b in writes:
                b.w = (dsem, cnt)
                b.r = {}

    def barrier(self):
        for cons in ("pe", "act", "dve", "pool", "sp"):
            for prod in self.sem:
                if prod != cons and self.tick[prod] > 0:
                    self._wait(cons, prod, self.tick[prod])
            for dn, (s, cnt) in self.dsem.items():
                if cnt > 0:
                    self._wait(cons, dn, cnt)

    def final_wait(self, dsems):
        for dn in dsems:
            s, cnt = self.dsem[dn]
            self._wait("sp", dn, cnt)


def build_program(debug=None):
    nc = bass.Bass("TRN2", target_bir_lowering=False)

    def din(name, shape, dt=F32):
        return nc.dram_tensor(name, list(shape), dt, kind="ExternalInput").ap()

    xT = din("xT", [D, TT])
    gains_d = din("gains", [128, 6, 8])
    ident_d = din("ident", [128, 128])
    w_in_d = din("w_in", [D, 3 * D])
    cw_d = din("cw", [128, 8, 3])
    w_out_d = din("w_out", [D, D])
    wkd_d = din("wkd", [D, 512])
    wkpd_d = din("wkpd", [D, 512])
    wv_d = din("wv", [D, 256])
    wq_d = din("wq", [D, D])
    wqp_d = din("wqp", [D, D])
    wo_d = din("wo", [D, D])
    sinkrow_d = din("sinkrow", [1, 2048])
    vsink_d = din("vsink", [1, 128])
    mask_d = din("mask", [128, 512])
    cos_d = din("cos", [128, NKV])
    sin_d = din("sin", [128, NKV])
    kbias_d = din("kbias", [128, NKB])
    wr_d = din("wr", [2, D, 36])
    rb_d = din("rb", [2, 128, 36])
    wgu_d = din("wgu", [2, NE, 128, 2 * 8 * 256])
    wdn_d = din("wdn", [2, NE, 128, 2 * 1024])
    outT = nc.dram_tensor("outT", [D, OWN], F32, kind="ExternalOutput").ap()
    scr = [nc.dram_tensor("scr%d" % l, [NE, TT], BF16).ap() for l in range(2)]
    if debug:
        dbg_h = nc.dram_tensor("dbg_h", [D, TT], F32, kind="ExternalOutput").ap()

    with ExitStack() as es:
        tr = Tracker(nc, es, same_engine_sync=SAME_ENGINE_SYNC)

        _cnt = [0]

        def sb(stack, name, shape, dt):
            _cnt[0] += 1
            return stack.enter_context(nc.sbuf_tensor("sb%d_%s" % (_cnt[0], name), list(shape), dt))

        h = sb(es, "h", [128, NCH, TT], F32)
        hB = [Buf("h%d" % i) for i in range(9)]
        gains = sb(es, "gains_sb", [128, 6, 8], F32)
        ident = sb(es, "ident_sb", [128, 128], F32)
        onesb = sb(es, "onesb", [128, 128], BF16)
        epsb = sb(es, "epsb", [128, 1], F32)
        cB = Buf("consts")
        psum = [es.enter_context(nc.psum_tensor("psb%d" % i, [128, 512], F32)) for i in range(8)]
        PB = [Buf("ps%d" % i) for i in range(8)]

        def ph(bank, half, n=256, parts=128):
            return psum[bank][0:parts, half * 256: half * 256 + n]

        xT3 = xT.rearrange("(c p) t -> p c t", p=128)
        tr.dma("sp", "init", [
            (h[:, :, 0:HALO + TW], xT3[:, :, 0:HALO + TW], (), hB[0:2]),
            (gains[:], gains_d[:], (), [cB]),
            (ident[:], ident_d[:], (), [cB]),
        ])
        tr.op("dve", lambda e: e.memset(onesb[:], 1.0 / D), writes=[cB])
        tr.op("dve", lambda e: e.memset(epsb[:], EPS), writes=[cB])

        def rmsnorm_stages(st, t0, n, gi, out_fn, xnB, scratch):
            sq, sqB, rstd, tB = scratch
            hb = hB[tile_idx(t0)]
            bank = st["ss"]

            def SQ(lo_, hi_):
                for c in range(lo_, hi_):
                    k = c % 4
                    tr.op("act", lambda e: e.activation(out=sq[k][:, :n], in_=h[:, c, t0:t0 + n], func=AF.Square),
                          reads=[hb], writes=[sqB[k]])

            def SS(lo_, hi_):
                for c in range(lo_, hi_):
                    k = c % 4
                    tr.op("pe", lambda e: e.matmul(ph(bank, 0, n), lhsT=onesb[:], rhs=sq[k][:, :n],
                                                   start=(c == 0), stop=(c == NCH - 1)),
                          reads=[sqB[k], cB], writes=[PB[bank]])

            def RS():
                tr.op("act", lambda e: e.activation(out=rstd[:, :n], in_=ph(bank, 0, n), func=AF.Ln,
                                                    bias=epsb[:, 0:1], scale=1.0),
                      reads=[cB], writes=[tB, PB[bank]])
                tr.op("act", lambda e: e.activation(out=rstd[:, :n], in_=rstd[:, :n], func=AF.Exp, scale=-0.5),
                      reads=[], writes=[tB])

            def XN():
                for c in range(NCH):
                    tr.op("dve", lambda e: e.scalar_tensor_tensor(out=out_fn(c), in0=h[:, c, t0:t0 + n],
                                                                  scalar=gains[:, gi, c:c + 1], in1=rstd[:, :n],
                                                                  op0=ALU.mult, op1=ALU.mult),
                          reads=[hb, tB, cB], writes=[xnB])

            return [lambda: SQ(0, 4), lambda: SS(0, 4), lambda: SQ(4, 8), lambda: (SS(4, 8), RS()), XN]

        def rmsnorm(st, t0, n, gi, out_fn, xnB, scratch):
            for f_ in rmsnorm_stages(st, t0, n, gi, out_fn, xnB, scratch):
                f_()

        def norm_scratch(stack):
            sq = [sb(stack, "sq%d" % i, [128, TW], BF16) for i in range(4)]
            sqB = [Buf("sq%d" % i) for i in range(4)]
            rstd = sb(stack, "rstd", [128, TW], F32)
            return (sq, sqB, rstd, Buf("nrm"))

        def wload(dst, src, wbuf):
            return [(dst[:, c, :], src[c * 128:(c + 1) * 128, :], (), [wbuf]) for c in range(NCH)]

        def dump_h():
            tr.barrier()
            tr.dma("sp", "dbg", [(dbg_h.rearrange("(c p) t -> p c t", p=128), h[:, :, :], hB, ())])
            tr.final_wait(["dbg"])

        def phase_conv():
            with ExitStack() as st_:
                w_in = sb(st_, "w_in", [128, NCH, 3 * D], BF16)
                w_out = sb(st_, "w_out", [128, NCH, D], BF16)
                cw = sb(st_, "cw", [128, 8, 3], F32)
                wB = Buf("convw")
                wB2 = Buf("convw2")
                tr.dma("sp", "wA2", [(cw[:], cw_d[:], (), [wB])])
                tr.dma("pool", "wA", wload(w_in, w_in_d, wB))
                tr.dma("pool", "xTb", [(h[:, :, HALO + TW * k:HALO + TW * (k + 1)], xT3[:, :, HALO + TW * k:HALO + TW * (k + 1)],
                                        (), [hB[1 + k]]) for k in range(1, OWN // TW)])
                tr.dma("pool", "wAo", wload(w_out, w_out_d, wB2))
                ns = norm_scratch(st_)
                xn = [sb(st_, "xnA%d" % i, [128, NCH, TW], BF16) for i in range(2)]
                xnB = [Buf("xnA%d" % i) for i in range(2)]
                gcs = [sb(st_, "gcs%d" % i, [128, TW], F32) for i in range(2)]
                gcB = [Buf("gcs%d" % i) for i in range(2)]
                u = sb(st_, "u", [128, NCH, TW + 2], F32)
                uB = [Buf("u%d" % j) for j in range(NCH)]
                c1 = [sb(st_, "c1_%d" % i, [128, TW], F32) for i in range(4)]
                c2 = [sb(st_, "c2_%d" % i, [128, TW], F32) for i in range(4)]
                c3 = [sb(st_, "c3_%d" % i, [128, TW], F32) for i in range(4)]
                gbs = [sb(st_, "gbs_%d" % i, [128, TW], F32) for i in range(4)]
                c1B = [Buf("c1_%d" % i) for i in range(4)]
                c2B = [Buf("c2_%d" % i) for i in range(4)]
                c3B = [Buf("c3_%d" % i) for i in range(4)]
                gbB = [Buf("gbs_%d" % i) for i in range(4)]
                y = [sb(st_, "y%d" % i, [128, NCH, TW], BF16) for i in range(2)]
                yB = [Buf("y%d" % i) for i in range(2)]
                tr.op("dve", lambda e: e.memset(u[:, :, 0:2], 0.0), writes=uB)
                psA = {"ss": 6}
                tiles = L0_CONV_TILES

                def NORM(ti):
                    t0, n = tiles[ti]
                    return rmsnorm_stages(psA, t0, n, 0, lambda c: xn[ti % 2][:, c, :n], xnB[ti % 2], ns)

                def J_pe(ti, j):
                    t0, n = tiles[ti]
                    p2 = ti % 2
                    s3 = j % 3
                    bA, bB = 2 * s3, 2 * s3 + 1
                    for part, (bank, half) in enumerate(((bA, 0), (bA, 1), (bB, 0))):
                        for c in range(NCH):
                            tr.op("pe", lambda e: e.matmul(
                                ph(bank, half, n), lhsT=w_in[:, c, part * D + j * 128: part * D + (j + 1) * 128],
                                rhs=xn[p2][:, c, :n], start=(c == 0), stop=(c == NCH - 1)),
                                reads=[wB, xnB[p2]], writes=[PB[bank]])

                def E1(ti, j):
                    t0, n = tiles[ti]
                    s, q4 = j % 2, j % 4
                    bA, bB = 2 * (j % 3), 2 * (j % 3) + 1
                    tr.op("act", lambda e: e.activation(out=gcs[s][:, :n], in_=ph(bA, 1, n), func=AF.Copy),
                          reads=[], writes=[gcB[s], PB[bA]])
                    tr.op("act", lambda e: e.activation(out=gbs[q4][:, :n], in_=ph(bA, 0, n), func=AF.Copy),
                          reads=[], writes=[gbB[q4], PB[bA]])
                    tr.op("dve", lambda e: e.tensor_tensor(out=u[:, j, 2:2 + n], in0=ph(bB, 0, n),
                                                           in1=gcs[s][:, :n], op=ALU.mult),
                          reads=[gcB[s]], writes=[uB[j], PB[bB]])
                    tr.op("act", lambda e: e.activation(out=c1[q4][:, :n], in_=u[:, j, 0:n], func=AF.Copy,
                                                        scale=cw[:, j, 0:1]),
                          reads=[uB[j], wB], writes=[c1B[q4]])

                def E2(ti, j):
                    t0, n = tiles[ti]
                    q4 = j % 4
                    tr.op("dve", lambda e: e.scalar_tensor_tensor(out=c2[q4][:, :n], in0=u[:, j, 1:1 + n],
                                                                  scalar=cw[:, j, 1:2], in1=c1[q4][:, :n],
                                                                  op0=ALU.mult, op1=ALU.add),
                          reads=[uB[j], wB, c1B[q4]], writes=[c2B[q4]])

                def E3(ti, j):
                    t0, n = tiles[ti]
                    q4 = j % 4
                    tr.op("dve", lambda e: e.scalar_tensor_tensor(out=c3[q4][:, :n], in0=u[:, j, 2:2 + n],
                                                                  scalar=cw[:, j, 2:3], in1=c2[q4][:, :n],
                                                                  op0=ALU.mult, op1=ALU.add),
                          reads=[uB[j], wB, c2B[q4]], writes=[c3B[q4]])

                def E4(ti, j):
                    t0, n = tiles[ti]
                    p2, q4 = ti % 2, j % 4
                    tr.op("dve", lambda e: e.tensor_tensor(out=y[p2][:, j, :n], in0=gbs[q4][:, :n],
                                                           in1=c3[q4][:, :n], op=ALU.mult),
                          reads=[c3B[q4], gbB[q4]], writes=[yB[p2]])
                    tr.op("act", lambda e: e.activation(out=u[:, j, 0:2], in_=u[:, j, n:n + 2], func=AF.Copy),
                          reads=[], writes=[uB[j]])

                def WOUT(ti):
                    t0, n = tiles[ti]
                    p2 = ti % 2
                    hb = hB[tile_idx(t0)]
                    for jo in range(NCH):
                        bank = (7, 6)[jo % 2]
                        for c in range(NCH):
                            tr.op("pe", lambda e: e.matmul(ph(bank, 0, n), lhsT=w_out[:, c, jo * 128:(jo + 1) * 128],
                                                           rhs=y[p2][:, c, :n], start=(c == 0), stop=(c == NCH - 1)),
                                  reads=[wB2, yB[p2]], writes=[PB[bank]])
                        tr.op("dve", lambda e: e.tensor_tensor(out=h[:, jo, t0:t0 + n], in0=ph(bank, 0, n),
                                                               in1=h[:, jo, t0:t0 + n], op=ALU.add),
                              reads=[hb], writes=[hb, PB[bank]])

                for f_ in NORM(0):
                    f_()
                slot = {0: 0, 1: 1, 2: 2, 3: 3, 5: 4}
                seq = [(ti, j) for ti in range(len(tiles)) for j in range(NCH)]

                def lagged(k, fn, lag):
                    if 0 <= k - lag < len(seq):
                        fn(*seq[k - lag])

                for k, (ti, j) in enumerate(seq):
                    if j == 0:
                        nst = NORM(ti + 1) if ti + 1 < len(tiles) else None
                    J_pe(ti, j)
                    E1(ti, j)
                    lagged(k, E2, 1)
                    lagged(k, E3, 2)
                    lagged(k, E4, 3)
                    if nst is not None and j in slot:
                        nst[slot[j]]()
                    if j == NCH - 1 and ti >= 1:
                        WOUT(ti - 1)
                for k in range(len(seq), len(seq) + 3):
                    lagged(k, E2, 1)
                    lagged(k, E3, 2)
                    lagged(k, E4, 3)
                WOUT(len(tiles) - 1)
                tr.barrier()

        def phase_moe(l, tiles, gi, final_gi=None):
            with ExitStack() as st_:
                xnb = sb(st_, "xnb", [128, NCH, TT], BF16)
                xnbB = Buf("xnb")
                wr = sb(st_, "wr", [128, NCH, 36], F32)
                rb = sb(st_, "rb", [128, 36], F32)
                rB = Buf("routerw")
                tr.dma("sp", "wr%d" % l, [
                    (wr[:], wr_d[l].rearrange("(c p) f -> p c f", p=128), (), [rB]),
                    (rb[:], rb_d[l], (), [rB]),
                ])
                wgu = [sb(st_, "wgu%d" % i, [128, 2, NCH, 256], BF16) for i in range(2)]
                wdn = [sb(st_, "wdn%d" % i, [128, 2, D], BF16) for i in range(2)]
                wB = [Buf("wexp%d" % i) for i in range(2)]

                def load_expert(e):
                    p = e % 2
                    tr.dma("pool", "wE%d" % p, [
                        (wgu[p][:].rearrange("p a c f -> p (a c f)"), wgu_d[l, e], (), [wB[p]]),
                        (wdn[p][:].rearrange("p a d -> p (a d)"), wdn_d[l, e], (), [wB[p]]),
                    ])
                load_expert(0)
                load_expert(1)
                ns = norm_scratch(st_)
                xn32_ = [sb(st_, "xn32_%d" % i, [128, NCH, TW], F32) for i in range(2)]
                xn32B_ = [Buf("xn32_%d" % i) for i in range(2)]
                NB = sum(n // 128 for (_, n) in tiles)
                R = {k: sb(st_, "r_" + k, [128, NB, w], F32) for k, w in
                     (("lg", 36), ("gmax", 1), ("gmask", 4), ("gsh", 4), ("gsum", 1), ("pen", 4),
                      ("em", 32), ("v1", 1), ("v2", 1), ("sel", 32), ("ex", 32), ("ssum", 1),
                      ("den", 1), ("comb", 32))}
                lgT = [sb(st_, "lgT%d" % i, [36, TW], F32) for i in range(2)]
                lgTB = [Buf("lgT%d" % i) for i in range(2)]
                ctile = [sb(st_, "ctile%d" % i, [128, 128], BF16) for i in range(2)]
                ctB = [Buf("ctile%d" % i) for i in range(2)]
                rtB = Buf("rtmp")
                psB = {"ss": 3}
                blk_cols = []
                for (t0, n) in tiles:
                    for b0 in range(0, n, 128):
                        blk_cols.append(t0 + b0)

                def NSTG(ti_):
                    t0, n = tiles[ti_]
                    x32 = xn32_[ti_ % 2]
                    return rmsnorm_stages(psB, t0, n, gi, lambda c: x32[:, c, :n], xn32B_[ti_ % 2], ns)

                def ROUTER(ti_):
                    t0, n = tiles[ti_]
                    xn32, xn32B = xn32_[ti_ % 2], xn32B_[ti_ % 2]
                    tr.op("act", lambda e: e.activation(out=xnb[:, :, t0:t0 + n], in_=xn32[:, :, :n], func=AF.Copy),
                          reads=[xn32B], writes=[xnbB])
                    lt, ltB = lgT[ti_ % 2], lgTB[ti_ % 2]
                    for c in range(NCH):
                        tr.op("pe", lambda e: e.matmul(psum[2][0:36, 0:n], lhsT=wr[:, c, :], rhs=xn32[:, c, :n],
                                                       start=(c == 0), stop=(c == NCH - 1)),
                              reads=[xn32B, rB], writes=[PB[2]])
                    tr.op("act", lambda e: e.activation(out=lt[:, :n], in_=psum[2][0:36, 0:n], func=AF.Copy),
                          reads=[], writes=[ltB, PB[2]])
                    for b0 in range(0, n, 128):
                        blk = blk_cols.index(t0 + b0)
                        bank = (6, 7)[blk % 2]
                        tr.op("pe", lambda e: e.transpose(out=psum[bank][:, 0:36], in_=lt[:, b0:b0 + 128],
                                                          identity=ident[0:36, 0:36]),
                              reads=[ltB, cB], writes=[PB[bank]])
                        tr.op("dve", lambda e: e.tensor_tensor(out=R["lg"][:, blk, :], in0=psum[bank][:, 0:36], in1=rb[:],
                                                               op=ALU.add),
                              reads=[rB], writes=[rtB, PB[bank]])

                for f_ in NSTG(0):
                    f_()
                for ti_ in range(len(tiles)):
                    nst = NSTG(ti_ + 1) if ti_ + 1 < len(tiles) else None
                    if nst is not None:
                        for f_ in nst[0:4]:
                            f_()
                    ROUTER(ti_)
                    if nst is not None:
                        nst[4]()
                V = lambda fn: tr.op("dve", fn, reads=[rtB], writes=[rtB])
                A = lambda fn: tr.op("act", fn, reads=[rtB], writes=[rtB])
                bc = lambda ap, w: ap.to_broadcast([128, NB, w])
                gl = R["lg"][:, :, 0:4]
                el = R["lg"][:, :, 4:36]
                V(lambda e: e.tensor_reduce(out=R["gmax"][:], in_=gl, axis=AX.X, op=ALU.max))
                V(lambda e: e.tensor_tensor(out=R["gmask"][:], in0=gl, in1=bc(R["gmax"][:], 4), op=ALU.is_ge))
                V(lambda e: e.tensor_tensor(out=R["gsh"][:], in0=gl, in1=bc(R["gmax"][:], 4), op=ALU.subtract))
                A(lambda e: e.activation(out=R["gsh"][:], in_=R["gsh"][:], func=AF.Exp))
                V(lambda e: e.tensor_reduce(out=R["gsum"][:], in_=R["gsh"][:], axis=AX.X, op=ALU.add))
                V(lambda e: e.tensor_scalar(out=R["pen"][:], in0=R["gmask"][:], scalar1=-1.0, scalar2=1e30,
                                            op0=ALU.add, op1=ALU.mult))
                V(lambda e: e.tensor_tensor(out=R["em"][:].rearrange("p b (g x) -> p b g x", g=4),
                                            in0=el.rearrange("p b (g x) -> p b g x", g=4),
                                            in1=R["pen"][:].unsqueeze(3).to_broadcast([128, NB, 4, 8]), op=ALU.add))
                V(lambda e: e.tensor_reduce(out=R["v1"][:], in_=R["em"][:], axis=AX.X, op=ALU.max))
                V(lambda e: e.tensor_tensor(out=R["sel"][:], in0=R["em"][:], in1=bc(R["v1"][:], 32), op=ALU.is_ge))
                V(lambda e: e.scalar_tensor_tensor(out=R["ex"][:], in0=R["sel"][:], scalar=-1e30, in1=R["em"][:],
                                                   op0=ALU.mult, op1=ALU.add))
                V(lambda e: e.tensor_reduce(out=R["v2"][:], in_=R["ex"][:], axis=AX.X, op=ALU.max))
                V(lambda e: e.tensor_tensor(out=R["sel"][:], in0=R["em"][:], in1=bc(R["v2"][:], 32), op=ALU.is_ge))
                V(lambda e: e.tensor_tensor(out=R["ex"][:], in0=R["em"][:], in1=bc(R["v1"][:], 32), op=ALU.subtract))
                A(lambda e: e.activation(out=R["ex"][:], in_=R["ex"][:], func=AF.Exp))
                V(lambda e: e.tensor_tensor(out=R["ex"][:], in0=R["ex"][:], in1=R["sel"][:], op=ALU.mult))
                V(lambda e: e.tensor_reduce(out=R["ssum"][:], in_=R["ex"][:], axis=AX.X, op=ALU.add))
                V(lambda e: e.tensor_tensor(out=R["den"][:], in0=R["gsum"][:], in1=R["ssum"][:], op=ALU.mult))
                V(lambda e: e.reciprocal(out=R["den"][:], in_=R["den"][:]))
                V(lambda e: e.tensor_tensor(out=R["comb"][:], in0=R["ex"][:], in1=bc(R["den"][:], 32), op=ALU.mult))
                c0 = tiles[0][0]
                scrB = [Buf("scr0"), Buf("scr1")]
                for gi_, b_ in enumerate(range(0, NB, 4)):
                    nb_ = min(4, NB - b_)
                    bank = (4, 5)[gi_ % 2]
                    ct = ctile[gi_ % 2]
                    tr.op("pe", lambda e: e.transpose(out=psum[bank][0:32 * nb_, 0:128],
                                                      in_=R["comb"][:, b_:b_ + nb_, :].rearrange("p b x -> p (b x)"),
                                                      identity=ident[:]),
                          reads=[rtB, cB], writes=[PB[bank]])
                    tr.op("act", lambda e: e.activation(out=ct[0:32 * nb_, :], in_=psum[bank][0:32 * nb_, 0:128], func=AF.Copy),
                          reads=[], writes=[ctB[gi_ % 2], PB[bank]])
                    tr.dma("sp", "scr%d_%d" % (l, gi_ % 2),
                           [(scr[l][:, blk_cols[b_ + k]:blk_cols[b_ + k] + 128], ct[32 * k:32 * k + 32, :], [ctB[gi_ % 2]], [scrB[gi_ % 2]])
                            for k in range(nb_)])
                cbc = [sb(st_, "cbc%d" % i, [128, TT], BF16) for i in range(2)]
                cbcB = [Buf("cbc%d" % i) for i in range(2)]
                sbuf_ = [sb(st_, "sil%d" % i, [128, 2, TW], BF16) for i in range(2)]
                tbuf = [sb(st_, "tb%d" % i, [128, 2, TW], BF16) for i in range(2)]
                hdn = [sb(st_, "hdn%d" % i, [128, 2, TW], BF16) for i in range(2)]
                sB = [Buf("sil%d" % i) for i in range(2)]
                tB_ = [Buf("tb%d" % i) for i in range(2)]
                hdB = [Buf("hdn%d" % i) for i in range(2)]
                gu_banks = [(0, 1), (2, 3)]

                def gu_ap(set_, q, n):
                    bank = gu_banks[set_][q // 2]
                    return ph(bank, q % 2, n)

                def gu_bufs(set_):
                    b0_, b1_ = gu_banks[set_]
                    return [PB[b0_], PB[b0_], PB[b1_], PB[b1_]]

                def gu_view(set_, which, n):
                    bank = gu_banks[set_][which]
                    return psum[bank][:, :].rearrange("p (a b) -> p a b", a=2)[:, :, :n]

                def load_cbc(e):
                    p = e % 2
                    tr.dma("sp", "cbc%d" % p, [(cbc[p][:, c0:TT], scr[l][e:e + 1, c0:TT].partition_broadcast(128),
                                               scrB, [cbcB[p]])])

                if final_gi is None:
                    steps = [(e, t0, n) for e in range(NE) for (t0, n) in tiles]
                else:
                    tail = []
                    for k in range(len(tiles) + 2):
                        if k < len(tiles):
                            tail.append((NE - 2,) + tiles[k])
                        if k >= 2:
                            tail.append((NE - 1,) + tiles[k - 2])
                    steps = [(e, t0, n) for e in range(NE - 2) for (t0, n) in tiles] + tail

                def GU(si):
                    e, t0, n = steps[si]
                    set_, p = si % 2, e % 2
                    for q in range(4):
                        for c in range(NCH):
                            tr.op("pe", lambda en: en.matmul(gu_ap(set_, q, n),
                                                             lhsT=wgu[p][:, q // 2, c, (q % 2) * 128:(q % 2 + 1) * 128],
                                                             rhs=xnb[:, c, t0:t0 + n], start=(c == 0), stop=(c == NCH - 1)),
                                  reads=[wB[p], xnbB], writes=[gu_bufs(set_)[q]])
                    gb = gu_bufs(set_)
                    tr.op("act", lambda en: en.activation(out=sbuf_[set_][:, :, :n], in_=gu_view(set_, 0, n), func=AF.Silu),
                          reads=[], writes=[sB[set_], gb[0]])
                    for f in range(2):
                        tr.op("dve", lambda en: en.tensor_tensor(out=tbuf[set_][:, f, :n], in0=sbuf_[set_][:, f, :n],
                                                                 in1=cbc[p][:, t0:t0 + n], op=ALU.mult),
                              reads=[sB[set_], cbcB[p]], writes=[tB_[set_]])
                    tr.op("dve", lambda en: en.tensor_tensor(out=hdn[set_][:, :, :n], in0=gu_view(set_, 1, n),
                                                             in1=tbuf[set_][:, :, :n], op=ALU.mult),
                          reads=[tB_[set_]], writes=[hdB[set_], gb[2]])


                def DN(si):
                    e, t0, n = steps[si]
                    set_, p = si % 2, e % 2
                    for jo in range(NCH):
                        bank, half = 4 + jo // 2, jo % 2
                        for f in range(2):
                            tr.op("pe", lambda en: en.matmul(ph(bank, half, n), lhsT=wdn[p][:, f, jo * 128:(jo + 1) * 128],
                                                             rhs=hdn[set_][:, f, :n], start=(f == 0), stop=(f == 1)),
                                  reads=[wB[p], hdB[set_]], writes=[PB[bank]])
                    hb = hB[tile_idx(t0)]
                    for b in range(4):
                        tr.op("dve", lambda en: en.tensor_tensor(
                            out=h[:, 2 * b:2 * b + 2, t0:t0 + n],
                            in0=psum[4 + b][:, :].rearrange("p (a b) -> p a b", a=2)[:, :, :n],
                            in1=h[:, 2 * b:2 * b + 2, t0:t0 + n], op=ALU.add),
                            reads=[hb], writes=[hb, PB[4 + b]])

                nt = len(tiles)
                if final_gi is not None:
                    fns = norm_scratch(st_)
                    fob = [sb(st_, "ob%d" % i, [128, NCH, TW], F32) for i in range(2)]
                    fobB = [Buf("ob%d" % i) for i in range(2)]
                    o3 = outT.rearrange("(c p) t -> p c t", p=128)

                    def FINAL(ti):
                        t0, n = tiles[ti]
                        p2 = ti % 2
                        rmsnorm({"ss": 3}, t0, n, final_gi, lambda c: fob[p2][:, c, :n], fobB[p2], fns)
                        tr.dma("sp", "out%d" % p2, [(o3[:, :, t0 - HALO:t0 - HALO + n], fob[p2][:, :, :n], [fobB[p2]], ())])
                load_cbc(0)
                GU(0)
                n_major = NE if final_gi is None else NE - 2
                tile_of = {t0: i for i, (t0, _) in enumerate(tiles)}
                for si in range(len(steps)):
                    e, t0_, _n = steps[si]
                    if si < n_major * nt and si % nt == 0:
                        if e + 1 < NE:
                            load_cbc(e + 1)
                        if e >= 1 and e + 1 < NE:
                            load_expert(e + 1)
                    if final_gi is not None and si == n_major * nt:
                        load_cbc(NE - 1)
                        load_expert(NE - 1)
                    if si + 1 < len(steps):
                        GU(si + 1)
                    DN(si)
                    if final_gi is not None and e == NE - 1 and tile_of[t0_] >= 1:
                        FINAL(tile_of[t0_] - 1)
                if final_gi is not None:
                    FINAL(nt - 1)
                    tr.final_wait(["out0", "out1"])
                tr.barrier()

        def phase_attn(stop_after_kv=False):
            with ExitStack() as st_:
                KT = sb(st_, "KT", [128, 4, NKV], BF16)
                Vx = sb(st_, "Vx", [128, NKB, 4, 128], BF16)
                cosb = sb(st_, "cosb", [128, NKV], BF16)
                sinb = sb(st_, "sinb", [128, NKV], BF16)
                kvalid = sb(st_, "kvalid", [128, NKB], F32)
                esrow = sb(st_, "esrow", [1, 2048], BF16)
                vsink = sb(st_, "vsink", [1, 128], BF16)
                mask = sb(st_, "mask", [128, 512], BF16)
                identb = sb(st_, "identb", [128, 128], BF16)
                ones4 = sb(st_, "ones4", [128, 4, 64], BF16)
                tabB = Buf("tables")
                KB_ = [Buf("K%d" % i) for i in range(NKB)]
                VB_ = [Buf("V%d" % i) for i in range(NKB)]
                tr.dma("pool", "tab", [
                    (cosb[:], cos_d[:], (), [tabB]),
                    (sinb[:], sin_d[:], (), [tabB]),
                    (vsink[:], vsink_d[:], (), [tabB]),
                    (mask[:], mask_d[:], (), [tabB]),
                    (identb[:], ident_d[:], (), [tabB]),
                ])
                wq = sb(st_, "wq", [128, NCH, D], BF16)
                wqp = sb(st_, "wqp", [128, NCH, D], BF16)
                wo = sb(st_, "wo", [128, NCH, D], BF16)
                wqB = Buf("wqo")
                woB = Buf("wo")
                ns = norm_scratch(st_)
                xn = [sb(st_, "xnC%d" % i, [128, NCH, TW], BF16) for i in range(2)]
                xnB = [Buf("xnC%d" % i) for i in range(2)]
                t1 = [sb(st_, "t1_%d" % i, [128, TW], F32) for i in range(2)]
                t2 = [sb(st_, "t2_%d" % i, [128, TW], F32) for i in range(2)]
                t1B = [Buf("t1_%d" % i) for i in range(2)]
                t2B = [Buf("t2_%d" % i) for i in range(2)]
                psC = {"ss": 5}

                def rope_proj(w_a, w_b, wBuf, col, xn_ap, xB, n, tcol, out_ap, outB, s):
                    for (w_, bank) in ((w_a, 2 * s), (w_b, 2 * s + 1)):
                        for c in range(NCH):
                            tr.op("pe", lambda e: e.matmul(ph(bank, 0, n), lhsT=w_[:, c, col:col + 128], rhs=xn_ap(c),
                                                           start=(c == 0), stop=(c == NCH - 1)),
                                  reads=[wBuf, xB], writes=[PB[bank]])
                    tr.op("dve", lambda e: e.tensor_tensor(out=t1[s][:, :n], in0=ph(2 * s, 0, n), in1=cosb[:, tcol:tcol + n],
                                                           op=ALU.mult), reads=[tabB], writes=[t1B[s], PB[2 * s]])
                    tr.op("dve", lambda e: e.tensor_tensor(out=t2[s][:, :n], in0=ph(2 * s + 1, 0, n), in1=sinb[:, tcol:tcol + n],
                                                           op=ALU.mult), reads=[tabB], writes=[t2B[s], PB[2 * s + 1]])
                    return lambda: tr.op("dve", lambda e: e.tensor_tensor(out=out_ap, in0=t1[s][:, :n], in1=t2[s][:, :n],
                                                                          op=ALU.add),
                                         reads=[t1B[s], t2B[s]], writes=outB)

                with ExitStack() as s1:
                    wkd = sb(s1, "wkd", [128, NCH, 512], BF16)
                    wkpd = sb(s1, "wkpd", [128, NCH, 512], BF16)
                    wv = sb(s1, "wv", [128, NCH, 256], BF16)
                    wB = Buf("wkv")
                    tr.dma("pool", "wC1", wload(wkd, wkd_d, wB) + wload(wkpd, wkpd_d, wB) + wload(wv, wv_d, wB))
                    tr.dma("pool", "wC2", wload(wq, wq_d, wqB) + wload(wqp, wqp_d, wqB))
                    tr.dma("pool", "wC2o", wload(wo, wo_d, woB))
                    with ExitStack() as s0:
                        esrow32 = sb(s0, "esrow32", [1, 1024], F32)
                        e32B = Buf("esrow32")
                        tr.dma("sp", "tab2", [(kvalid[:], kbias_d[:], (), [tabB])])
                        for hf in range(2):
                            tr.dma("sp", "tab3", [(esrow32[:], sinkrow_d[:, hf * 1024:(hf + 1) * 1024], (), [e32B])])
                            tr.op("act", lambda e: e.activation(out=esrow[:, hf * 1024:(hf + 1) * 1024], in_=esrow32[:],
                                                                func=AF.Exp), reads=[e32B], writes=[tabB])
                    tr.op("dve", lambda e: e.memset(ones4[:], 1.0), writes=[tabB])
                    for kb in range(NKB):
                        tr.op("act", lambda e: e.activation(out=Vx[:, kb, :, 64:128], in_=ones4[:], func=AF.Copy,
                                                            scale=kvalid[:, kb:kb + 1]),
                              reads=[tabB], writes=[VB_[kb]])
                    tiles = L0_MOE_TILES

                    def NORM1(ti):
                        t0, n = tiles[ti]
                        return rmsnorm_stages(psC, t0, n, 2, lambda c: xn[ti % 2][:, c, :n], xnB[ti % 2], ns)

                    for f_ in NORM1(0):
                        f_()
                    for ti, (t0, n) in enumerate(tiles):
                        p2 = ti % 2
                        kbs = [KB_[(t0 - 2) // 128 + i] for i in range(n // 128)]
                        nst = NORM1(ti + 1) if ti + 1 < len(tiles) else None
                        pend = None
                        for g in range(4):
                            fin = rope_proj(wkd, wkpd, wB, g * 128, lambda c: xn[p2][:, c, :n], xnB[p2], n, t0 - 2,
                                            KT[:, g, t0 - 2:t0 - 2 + n], kbs, g % 2)
                            if pend is not None:
                                pend()
                            pend = fin
                            if nst is not None:
                                if g == 0:
                                    nst[0](); nst[1]()
                                elif g == 1:
                                    nst[2](); nst[3]()
                                elif g == 2:
                                    nst[4]()
                        pend()
                        for b0 in range(0, n, 128):
                            kb = (t0 + b0 - 2) // 128
                            bank = 6 + kb % 2
                            for c in range(NCH):
                                tr.op("pe", lambda e: e.matmul(ph(bank, 0, 256), lhsT=xn[p2][:, c, b0:b0 + 128], rhs=wv[:, c, :],
                                                               start=(c == 0), stop=(c == NCH - 1)),
                                      reads=[wB, xnB[p2]], writes=[PB[bank]])
                            tr.op("act", lambda e: e.activation(out=Vx[:, kb, :, 0:64],
                                                                in_=ph(bank, 0, 256).rearrange("p (g d) -> p g d", g=4),
                                                                func=AF.Copy, scale=kvalid[:, kb:kb + 1]),
                                  reads=[tabB], writes=[VB_[kb], PB[bank]])
                    tr.barrier()
                if stop_after_kv:
                    return
                with ExitStack() as s2:
                    wB = wqB
                    wB2 = woB
                    QT = [sb(s2, "QT%d" % i, [128, NCH, TW], BF16) for i in range(2)]
                    QB = [Buf("QT%d" % i) for i in range(2)]
                    OT = [sb(s2, "OT%d" % i, [128, NCH, TW], BF16) for i in range(2)]
                    OB = [Buf("OT%d" % i) for i in range(2)]
                    PT = [sb(s2, "PT%d" % i, [128, 1024], BF16) for i in range(2)]
                    PTB = [Buf("PT%d" % i) for i in range(2)]
                    rden = [sb(s2, "rdenA%d" % i, [64, 512], F32) for i in range(2)]
                    rdB = [Buf("rdenA%d" % i) for i in range(2)]
                    s_banks = [(0, 1), (2, 3)]
                    o_banks = [6, 7]
                    tiles = L1_TILES

                    def QNORM(ti):
                        t0, n = tiles[ti]
                        p2 = ti % 2
                        return rmsnorm_stages(psC, t0, n, 3, lambda c: xn[p2][:, c, :n], xnB[p2], ns)

                    def QPROJ(ti):
                        t0, n = tiles[ti]
                        p2 = ti % 2
                        pend = None
                        for c8 in range(NCH):
                            fin = rope_proj(wq, wqp, wB, c8 * 128, lambda c: xn[p2][:, c, :n], xnB[p2], n, t0 - 2,
                                            QT[p2][:, c8, :n], [QB[p2]], c8 % 2)
                            if pend is not None:
                                pend()
                            pend = fin
                        pend()

                    def S_stage(ti, ui):
                        t0, n = tiles[ti]
                        p2 = ti % 2
                        qbl, g = ui // 4, ui % 4
                        sset = ui % 2
                        kb_cur = (t0 + 128 * qbl - 2) // 128
                        for kbi, kb in enumerate((kb_cur - 1, kb_cur)):
                            for half in range(2):
                                lo = 64 * half
                                bank = s_banks[sset][half]
                                tr.op("pe", lambda e: e.matmul(
                                    psum[bank][:, kbi * 256:(kbi + 1) * 256].rearrange("p (a b) -> p a b", a=2),
                                    lhsT=KT[lo:lo + 64, g, kb * 128:(kb + 1) * 128],
                                    rhs=QT[p2][lo:lo + 64, 2 * g:2 * g + 2, 128 * qbl:128 * qbl + 128],
                                    start=True, stop=False),
                                    reads=[KB_[kb], QB[p2]], writes=[PB[bank]])
                            for half in range(2):
                                bank = s_banks[sset][half]
                                tr.op("pe", lambda e: e.matmul(
                                    psum[bank][:, kbi * 256:(kbi + 1) * 256], lhsT=identb[:],
                                    rhs=mask[:, kbi * 256:(kbi + 1) * 256], start=False, stop=True),
                                    reads=[tabB], writes=[PB[bank]])
                        for half in range(2):
                            bank = s_banks[sset][half]
                            tr.op("act", lambda e: e.activation(
                                out=PT[sset][:, :].rearrange("p (k h x) -> p k h x", k=2, h=2)[:, :, half, :],
                                in_=psum[bank][:, :].rearrange("p (k x) -> p k x", k=2),
                                func=AF.Exp, scale=0.125),
                                reads=[], writes=[PTB[sset], PB[bank]])

                    def PV_stage(ti, ui):
                        t0, n = tiles[ti]
                        p2 = ti % 2
                        qbl, g = ui // 4, ui % 4
                        sset = ui % 2
                        kb_cur = (t0 + 128 * qbl - 2) // 128
                        ob_ = o_banks[ui % 2]
                        ob = [PB[ob_]]
                        rd, rB_ = rden[ui % 2], rdB[ui % 2]
                        tr.op("pe", lambda e: e.matmul(psum[ob_][:, :], lhsT=Vx[:, kb_cur - 1, g, :], rhs=PT[sset][:, 0:512],
                                                       start=True, stop=False),
                              reads=[VB_[kb_cur - 1], PTB[sset]], writes=ob)
                        tr.op("pe", lambda e: e.matmul(psum[ob_][:, :], lhsT=Vx[:, kb_cur, g, :], rhs=PT[sset][:, 512:1024],
                                                       start=False, stop=False),
                              reads=[VB_[kb_cur], PTB[sset]], writes=ob)
                        tr.op("pe", lambda e: e.matmul(psum[ob_][:, :], lhsT=vsink[0:1, :], rhs=esrow[0:1, g * 512:(g + 1) * 512],
                                                       start=False, stop=True),
                              reads=[tabB], writes=ob)
                        tr.op("act", lambda e: e.activation(out=rd[:, :], in_=psum[ob_][64:128, :], func=AF.Ln),
                              reads=[], writes=[rB_] + ob)

                    def PV_fin(ti, ui):
                        t0, n = tiles[ti]
                        p2 = ti % 2
                        qbl, g = ui // 4, ui % 4
                        ob_ = o_banks[ui % 2]
                        ob = [PB[ob_]]
                        rd, rB_ = rden[ui % 2], rdB[ui % 2]
                        tr.op("act", lambda e: e.activation(out=rd[:, :], in_=rd[:, :], func=AF.Exp, scale=-1.0),
                              reads=[], writes=[rB_])
                        for half in range(2):
                            lo = 64 * half
                            tr.op("dve", lambda e: e.tensor_tensor(
                                out=OT[p2][lo:lo + 64, 2 * g:2 * g + 2, 128 * qbl:128 * qbl + 128],
                                in0=psum[ob_][0:64, half * 256:(half + 1) * 256].rearrange("p (a b) -> p a b", a=2),
                                in1=rd[:, half * 256:(half + 1) * 256].rearrange("p (a b) -> p a b", a=2),
                                op=ALU.mult),
                                reads=[rB_], writes=[OB[p2]] + ob)

                    def OPROJ(ti):
                        t0, n = tiles[ti]
                        p2 = ti % 2
                        hb = hB[tile_idx(t0)]
                        for jo in range(NCH):
                            bank = 4 + jo % 2
                            for c in range(NCH):
                                tr.op("pe", lambda e: e.matmul(ph(bank, 0, n), lhsT=wo[:, c, jo * 128:(jo + 1) * 128],
                                                               rhs=OT[p2][:, c, :n], start=(c == 0), stop=(c == NCH - 1)),
                                      reads=[wB2, OB[p2]], writes=[PB[bank]])
                            tr.op("dve", lambda e: e.tensor_tensor(out=h[:, jo, t0:t0 + n], in0=ph(bank, 0, n),
                                                                   in1=h[:, jo, t0:t0 + n], op=ALU.add),
                                  reads=[hb], writes=[hb, PB[bank]])

                    for f_ in QNORM(0):
                        f_()
                    QPROJ(0)
                    slot = {0: 0, 1: 1, 2: 2, 3: 3, 5: 4}
                    for ti in range(len(tiles)):
                        nu = (tiles[ti][1] // 128) * 4
                        nst = QNORM(ti + 1) if ti + 1 < len(tiles) else None
                        S_stage(ti, 0)
                        for ui in range(nu):
                            if ui + 1 < nu:
                                S_stage(ti, ui + 1)
                            PV_stage(ti, ui)
                            if ui >= 1:
                                PV_fin(ti, ui - 1)
                            if nst is not None and ui in slot:
                                nst[slot[ui]]()
                        PV_fin(ti, nu - 1)
                        if ti + 1 < len(tiles):
                            QPROJ(ti + 1)
                        OPROJ(ti)
                    tr.barrier()

        def phase_final():
            with ExitStack() as st_:
                ns = norm_scratch(st_)
                ob = [sb(st_, "ob%d" % i, [128, NCH, TW], F32) for i in range(2)]
                obB = [Buf("ob%d" % i) for i in range(2)]
                psE = {"ss": 3}
                o3 = outT.rearrange("(c p) t -> p c t", p=128)
                for ti, (t0, n) in enumerate(L1_TILES):
                    p2 = ti % 2
                    rmsnorm(psE, t0, n, 5, lambda c: ob[p2][:, c, :n], obB[p2], ns)
                    tr.dma("sp", "out%d" % p2, [(o3[:, :, t0 - HALO:t0 - HALO + n], ob[p2][:, :, :n], [obB[p2]], ())])
                tr.final_wait(["out0", "out1"])

        stages = [("init", lambda: None), ("conv", phase_conv), ("moe0", lambda: phase_moe(0, L0_MOE_TILES, 1)), ("attn", phase_attn),
                  ("moe1", lambda: phase_moe(1, L1_TILES, 4, final_gi=(None if debug else 5)))]
        if debug == "attn_kv":
            stages = [("conv", phase_conv), ("attn_kv", lambda: phase_attn(True))]
        if debug == "attn_only":
            stages = [("conv", phase_conv), ("attn_only", phase_attn)]
        done = False
        for name, fn in stages:
            fn()
            if debug == name:
                dump_h()
                done = True
                break
        if not done:
            if debug:
                phase_final()
                dump_h()
        else:
            with ExitStack() as st_:
                z = sb(st_, "zz", [128, NCH, TW], F32)
                zB = Buf("zz")
                tr.op("dve", lambda e: e.memset(z[:], 0.0), writes=[zB])
                o3 = outT.rearrange("(c p) t -> p c t", p=128)
                tr.dma("sp", "outz", [(o3[:, :, k * TW:(k + 1) * TW], z[:], [zB], ()) for k in range(OWN // TW)])
                tr.final_wait(["outz"])
    return nc


def _q_cols():
    cols, cols_p = [], []
    perm = (np.arange(64) + 32) % 64
    for c8 in range(8):
        g, i = c8 // 2, c8 % 2
        for hd in (4 * g + i, 4 * g + 2 + i):
            cols.append(hd * 64 + np.arange(64))
            cols_p.append(hd * 64 + perm)
    return np.concatenate(cols), np.concatenate(cols_p)


def prep_shared(inp):
    f = lambda a: np.ascontiguousarray(a, dtype=np.float32)
    S = {}
    g6 = np.stack([inp["conv_norm_g"][0], inp["ffn_norm_g"][0], inp["kv_norm_g"], inp["attn_norm_g"][0],
                   inp["ffn_norm_g"][1], inp["final_norm_g"]])
    S["gains"] = f(g6.reshape(6, 8, 128).transpose(2, 0, 1))
    S["ident"] = np.eye(128, dtype=np.float32)
    S["w_in"] = f(inp["conv_w_in"][0])
    S["cw"] = f(inp["conv_w"][0].reshape(3, 8, 128).transpose(2, 1, 0))
    S["w_out"] = f(inp["conv_w_out"][0])
    wk, wv = inp["w_kv"][:, :256], inp["w_kv"][:, 256:]
    perm = (np.arange(64) + 32) % 64
    kc = np.concatenate([np.concatenate([g * 64 + np.arange(64)] * 2) for g in range(4)])
    kcp = np.concatenate([np.concatenate([g * 64 + perm] * 2) for g in range(4)])
    S["wkd"] = f(wk[:, kc])
    S["wkpd"] = f(wk[:, kcp])
    S["wv"] = f(wv)
    qc, qcp = _q_cols()
    S["wq"] = f(inp["w_q"][0][:, qc])
    S["wqp"] = f(inp["w_q"][0][:, qcp])
    S["wo"] = f(inp["w_o"][0][qc, :])
    sk = inp["sinks"][0]
    row = np.zeros((4, 4, 128), np.float32)
    for g in range(4):
        for hh in range(4):
            half, i = hh // 2, hh % 2
            row[g, hh, :] = sk[4 * g + i + 2 * half]
    S["sinkrow"] = row.reshape(1, 2048)
    vs = np.zeros((1, 128), np.float32)
    vs[0, 64:] = 1.0
    S["vsink"] = vs
    k = np.arange(128)[:, None]
    q = np.arange(128)[None, :]
    m = np.zeros((128, 2, 2, 128), np.float32)
    m[:, 0] = np.where(k > q, 0.0, -30000.0).astype(np.float32)[:, None, :]
    m[:, 1] = np.where(k <= q, 0.0, -30000.0).astype(np.float32)[:, None, :]
    S["mask"] = m.reshape(128, 512)
    S["wr"] = f(np.concatenate([inp["router_group_w"], inp["router_expert_w"]], axis=2))
    rb = np.concatenate([inp["router_group_b"], inp["router_expert_b"]], axis=1)
    S["rb"] = f(np.broadcast_to(rb[:, None, :], (2, 128, 36)))
    wg = inp["w_gate"].reshape(2, NE, 8, 128, 256).transpose(0, 1, 3, 2, 4)
    wu = inp["w_up"].reshape(2, NE, 8, 128, 256).transpose(0, 1, 3, 2, 4)
    S["wgu"] = f(np.stack([wg, wu], axis=3).reshape(2, NE, 128, 2 * 8 * 256))
    S["wdn"] = f(inp["w_down"].reshape(2, NE, 2, 128, D).transpose(0, 1, 3, 2, 4).reshape(2, NE, 128, 2 * D))
    return S


def prep_core(inp, b, c):
    P0 = NMETA + c * OWN - HALO
    hfull = np.concatenate([inp["meta_tokens"].astype(np.float32), inp["x"][b]], axis=0)
    pos = P0 + np.arange(TT)
    rows = np.zeros((TT, D), np.float32)
    ok = pos >= 0
    rows[ok] = hfull[pos[ok]]
    C = {"xT": np.ascontiguousarray(rows.T)}
    half = 32
    inv = (10000.0 ** (-np.arange(half, dtype=np.float32) / half)).astype(np.float32)
    tp = pos[2:].astype(np.float32)
    ang = tp[None, :] * inv[np.arange(128) % 32][:, None]
    sign = np.where((np.arange(128) % 64) < 32, -1.0, 1.0).astype(np.float32)[:, None]
    C["cos"] = np.cos(ang).astype(np.float32)
    C["sin"] = (np.sin(ang) * sign).astype(np.float32)
    kpos = pos[2:].reshape(NKB, 128).T
    C["kbias"] = np.where(kpos >= 0, 1.0, 0.0).astype(np.float32)
    return C


_NC_CACHE = {}


def kernel(**inputs):
    inp = {k: np.asarray(v) for k, v in inputs.items()}
    S = prep_shared(inp)
    in_maps = []
    for core in range(8):
        b, c = core // 4, core % 4
        m = dict(S)
        m.update(prep_core(inp, b, c))
        in_maps.append(m)
    if "nc" not in _NC_CACHE:
        _NC_CACHE["nc"] = build_program()
    res = run_bass_kernel_spmd(_NC_CACHE["nc"], in_maps, core_ids=list(range(8)))
    out = np.empty((2, SEQ, D), np.float32)
    for core in range(8):
        b, c = core // 4, core % 4
        out[b, c * OWN:(c + 1) * OWN, :] = res.results[core]["outT"].T
    return out
```

```python
import numpy as np
from contextlib import ExitStack
import concourse.bass as bass
import concourse.mybir as mybir
from concourse.bass_utils import run_bass_kernel_spmd

F32 = mybir.dt.float32
BF16 = mybir.dt.bfloat16
ALU = mybir.AluOpType
AF = mybir.ActivationFunctionType
AX = mybir.AxisListType

D = 1024
NCH = 8
SEQ = 8192
NMETA = 16
HALO = 130
OWN = 2048
TT = HALO + OWN
NKV = TT - 2
NKB = NKV // 128
NE = 32
EPS = 1e-5
TW = 256
SAME_ENGINE_SYNC = True

L0_CONV_TILES = [(0, 130)] + [(HALO + TW * k, TW) for k in range(OWN // TW)]
L0_MOE_TILES = [(2, 128)] + [(HALO + TW * k, TW) for k in range(OWN // TW)]
L1_TILES = [(HALO + TW * k, TW) for k in range(OWN // TW)]


def tile_idx(t0):
    return 0 if t0 < HALO else 1 + (t0 - HALO) // TW


class Buf:
    __slots__ = ("name", "w", "r")

    def __init__(self, name):
        self.name = name
        self.w = None
        self.r = {}


class Tracker:
    def __init__(self, nc, es, same_engine_sync=True):
        self.nc = nc
        self.eng = {"pe": nc.tensor, "act": nc.scalar, "dve": nc.vector, "pool": nc.gpsimd, "sp": nc.sync}
        self.sem = {k: es.enter_context(nc.semaphore("s_" + k)) for k in ("pe", "act", "dve", "pool")}
        self.tick = {k: 0 for k in self.sem}
        self.waited = {}
        self.dsem = {}
        self.es = es
        self.same = same_engine_sync
        self.nwait = 0

    def _sem_of(self, prod):
        return self.sem[prod] if prod in self.sem else self.dsem[prod][0]

    def _wait(self, cons, prod, tick):
        if prod == cons and (cons == "pe" or not self.same):
            return
        key = (cons, prod)
        if self.waited.get(key, 0) >= tick:
            return
        self.waited[key] = tick
        self.eng[cons].wait_ge(self._sem_of(prod), tick)
        self.nwait += 1

    def deps(self, cons, reads, writes):
        need = {}
        for b in reads:
            if b.w is not None:
                need[b.w[0]] = max(need.get(b.w[0], 0), b.w[1])
        for b in writes:
            if b.w is not None:
                need[b.w[0]] = max(need.get(b.w[0], 0), b.w[1])
            for p, t in b.r.items():
                need[p] = max(need.get(p, 0), t)
        for p, t in need.items():
            self._wait(cons, p, t)

    def op(self, eng, fn, reads=(), writes=()):
        self.deps(eng, reads, writes)
        ins = fn(self.eng[eng])
        self.tick[eng] += 1
        t = self.tick[eng]
        ins.then_inc(self.sem[eng], 1)
        for b in reads:
            b.r[eng] = t
        for b in writes:
            b.w = (eng, t)
            b.r = {}
        return t

    def new_dsem(self, name):
        if name not in self.dsem:
            self.dsem[name] = [self.es.enter_context(self.nc.semaphore("d_" + name)), 0]

    def dma(self, queue, dsem, items):
        self.new_dsem(dsem)
        for (_, _, reads, writes) in items:
            self.deps(queue, reads, writes)
        sem = self.dsem[dsem][0]
        for (o, i, _, _) in items:
            self.eng[queue].dma_start(out=o, in_=i).then_inc(sem, 16)
            self.dsem[dsem][1] += 16
        cnt = self.dsem[dsem][1]
        for (_, _, reads, writes) in items:
            for b in reads:
                b.r[dsem] = cnt
            for b in writes:
                b.w = (dsem, cnt)
                b.r = {}

    def barrier(self):
        for cons in ("pe", "act", "dve", "pool", "sp"):
            for prod in self.sem:
                if prod != cons and self.tick[prod] > 0:
                    self._wait(cons, prod, self.tick[prod])
            for dn, (s, cnt) in self.dsem.items():
                if cnt > 0:
                    self._wait(cons, dn, cnt)

    def final_wait(self, dsems):
        for dn in dsems:
            s, cnt = self.dsem[dn]
            self._wait("sp", dn, cnt)


def build_program(debug=None):
    nc = bass.Bass("TRN2", target_bir_lowering=False)

    def din(name, shape, dt=F32):
        return nc.dram_tensor(name, list(shape), dt, kind="ExternalInput").ap()

    xT = din("xT", [D, TT])
    gains_d = din("gains", [128, 6, 8])
    ident_d = din("ident", [128, 128])
    w_in_d = din("w_in", [D, 3 * D])
    cw_d = din("cw", [128, 8, 3])
    w_out_d = din("w_out", [D, D])
    wkd_d = din("wkd", [D, 512])
    wkpd_d = din("wkpd", [D, 512])
    wv_d = din("wv", [D, 256])
    wq_d = din("wq", [D, D])
    wqp_d = din("wqp", [D, D])
    wo_d = din("wo", [D, D])
    sinkrow_d = din("sinkrow", [1, 2048])
    vsink_d = din("vsink", [1, 128])
    mask_d = din("mask", [128, 512])
    cos_d = din("cos", [128, NKV])
    sin_d = din("sin", [128, NKV])
    kbias_d = din("kbias", [128, NKB])
    wr_d = din("wr", [2, D, 36])
    rb_d = din("rb", [2, 128, 36])
    wgu_d = din("wgu", [2, NE, 128, 2 * 8 * 256])
    wdn_d = din("wdn", [2, NE, 128, 2 * 1024])
    outT = nc.dram_tensor("outT", [D, OWN], F32, kind="ExternalOutput").ap()
    scr = [nc.dram_tensor("scr%d" % l, [NE, TT], BF16).ap() for l in range(2)]
    if debug:
        dbg_h = nc.dram_tensor("dbg_h", [D, TT], F32, kind="ExternalOutput").ap()

    with ExitStack() as es:
        tr = Tracker(nc, es, same_engine_sync=SAME_ENGINE_SYNC)

        _cnt = [0]

        def sb(stack, name, shape, dt):
            _cnt[0] += 1
            return stack.enter_context(nc.sbuf_tensor("sb%d_%s" % (_cnt[0], name), list(shape), dt))

        h = sb(es, "h", [128, NCH, TT], F32)
        hB = [Buf("h%d" % i) for i in range(9)]
        gains = sb(es, "gains_sb", [128, 6, 8], F32)
        ident = sb(es, "ident_sb", [128, 128], F32)
        onesb = sb(es, "onesb", [128, 128], BF16)
        epsb = sb(es, "epsb", [128, 1], F32)
        cB = Buf("consts")
        psum = [es.enter_context(nc.psum_tensor("psb%d" % i, [128, 512], F32)) for i in range(8)]
        PB = [Buf("ps%d" % i) for i in range(8)]

        def ph(bank, half, n=256, parts=128):
            return psum[bank][0:parts, half * 256: half * 256 + n]

        xT3 = xT.rearrange("(c p) t -> p c t", p=128)
        tr.dma("sp", "init", [
            (h[:, :, 0:HALO + TW], xT3[:, :, 0:HALO + TW], (), hB[0:2]),
            (gains[:], gains_d[:], (), [cB]),
            (ident[:], ident_d[:], (), [cB]),
        ])
        tr.op("dve", lambda e: e.memset(onesb[:], 1.0 / D), writes=[cB])
        tr.op("dve", lambda e: e.memset(epsb[:], EPS), writes=[cB])

        def rmsnorm_stages(st, t0, n, gi, out_fn, xnB, scratch):
            sq, sqB, rstd, tB = scratch
            hb = hB[tile_idx(t0)]
            bank = st["ss"]

            def SQ(lo_, hi_):
                for c in range(lo_, hi_):
                    k = c % 4
                    tr.op("act", lambda e: e.activation(out=sq[k][:, :n], in_=h[:, c, t0:t0 + n], func=AF.Square),
                          reads=[hb], writes=[sqB[k]])

            def SS(lo_, hi_):
                for c in range(lo_, hi_):
                    k = c % 4
                    tr.op("pe", lambda e: e.matmul(ph(bank, 0, n), lhsT=onesb[:], rhs=sq[k][:, :n],
                                                   start=(c == 0), stop=(c == NCH - 1)),
                          reads=[sqB[k], cB], writes=[PB[bank]])

            def RS():
                tr.op("act", lambda e: e.activation(out=rstd[:, :n], in_=ph(bank, 0, n), func=AF.Ln,
                                                    bias=epsb[:, 0:1], scale=1.0),
                      reads=[cB], writes=[tB, PB[bank]])
                tr.op("act", lambda e: e.activation(out=rstd[:, :n], in_=rstd[:, :n], func=AF.Exp, scale=-0.5),
                      reads=[], writes=[tB])

            def XN():
                for c in range(NCH):
                    tr.op("dve", lambda e: e.scalar_tensor_tensor(out=out_fn(c), in0=h[:, c, t0:t0 + n],
                                                                  scalar=gains[:, gi, c:c + 1], in1=rstd[:, :n],
                                                                  op0=ALU.mult, op1=ALU.mult),
                          reads=[hb, tB, cB], writes=[xnB])

            return [lambda: SQ(0, 4), lambda: SS(0, 4), lambda: SQ(4, 8), lambda: (SS(4, 8), RS()), XN]

        def rmsnorm(st, t0, n, gi, out_fn, xnB, scratch):
            for f_ in rmsnorm_stages(st, t0, n, gi, out_fn, xnB, scratch):
                f_()

        def norm_scratch(stack):
            sq = [sb(stack, "sq%d" % i, [128, TW], BF16) for i in range(4)]
            sqB = [Buf("sq%d" % i) for i in range(4)]
            rstd = sb(stack, "rstd", [128, TW], F32)
            return (sq, sqB, rstd, Buf("nrm"))

        def wload(dst, src, wbuf):
            return [(dst[:, c, :], src[c * 128:(c + 1) * 128, :], (), [wbuf]) for c in range(NCH)]

        def dump_h():
            tr.barrier()
            tr.dma("sp", "dbg", [(dbg_h.rearrange("(c p) t -> p c t", p=128), h[:, :, :], hB, ())])
            tr.final_wait(["dbg"])

        def phase_conv():
            with ExitStack() as st_:
                w_in = sb(st_, "w_in", [128, NCH, 3 * D], BF16)
                w_out = sb(st_, "w_out", [128, NCH, D], BF16)
                cw = sb(st_, "cw", [128, 8, 3], F32)
                wB = Buf("convw")
                wB2 = Buf("convw2")
                tr.dma("sp", "wA2", [(cw[:], cw_d[:], (), [wB])])
                tr.dma("pool", "wA", wload(w_in, w_in_d, wB))
                tr.dma("pool", "xTb", [(h[:, :, HALO + TW * k:HALO + TW * (k + 1)], xT3[:, :, HALO + TW * k:HALO + TW * (k + 1)],
                                        (), [hB[1 + k]]) for k in range(1, OWN // TW)])
                tr.dma("pool", "wAo", wload(w_out, w_out_d, wB2))
                ns = norm_scratch(st_)
                xn = [sb(st_, "xnA%d" % i, [128, NCH, TW], BF16) for i in range(2)]
                xnB = [Buf("xnA%d" % i) for i in range(2)]
                gcs = [sb(st_, "gcs%d" % i, [128, TW], F32) for i in range(2)]
                gcB = [Buf("gcs%d" % i) for i in range(2)]
                u = sb(st_, "u", [128, NCH, TW + 2], F32)
                uB = [Buf("u%d" % j) for j in range(NCH)]
                c1 = [sb(st_, "c1_%d" % i, [128, TW], F32) for i in range(4)]
                c2 = [sb(st_, "c2_%d" % i, [128, TW], F32) for i in range(4)]
                c3 = [sb(st_, "c3_%d" % i, [128, TW], F32) for i in range(4)]
                gbs = [sb(st_, "gbs_%d" % i, [128, TW], F32) for i in range(4)]
                c1B = [Buf("c1_%d" % i) for i in range(4)]
                c2B = [Buf("c2_%d" % i) for i in range(4)]
                c3B = [Buf("c3_%d" % i) for i in range(4)]
                gbB = [Buf("gbs_%d" % i) for i in range(4)]
                y = [sb(st_, "y%d" % i, [128, NCH, TW], BF16) for i in range(2)]
                yB = [Buf("y%d" % i) for i in range(2)]
                tr.op("dve", lambda e: e.memset(u[:, :, 0:2], 0.0), writes=uB)
                psA = {"ss": 6}
                tiles = L0_CONV_TILES

                def NORM(ti):
                    t0, n = tiles[ti]
                    return rmsnorm_stages(psA, t0, n, 0, lambda c: xn[ti % 2][:, c, :n], xnB[ti % 2], ns)

                def J_pe(ti, j):
                    t0, n = tiles[ti]
                    p2 = ti % 2
                    s3 = j % 3
                    bA, bB = 2 * s3, 2 * s3 + 1
                    for part, (bank, half) in enumerate(((bA, 0), (bA, 1), (bB, 0))):
                        for c in range(NCH):
                            tr.op("pe", lambda e: e.matmul(
                                ph(bank, half, n), lhsT=w_in[:, c, part * D + j * 128: part * D + (j + 1) * 128],
                                rhs=xn[p2][:, c, :n], start=(c == 0), stop=(c == NCH - 1)),
                                reads=[wB, xnB[p2]], writes=[PB[bank]])

                def E1(ti, j):
                    t0, n = tiles[ti]
                    s, q4 = j % 2, j % 4
                    bA, bB = 2 * (j % 3), 2 * (j % 3) + 1
                    tr.op("act", lambda e: e.activation(out=gcs[s][:, :n], in_=ph(bA, 1, n), func=AF.Copy),
                          reads=[], writes=[gcB[s], PB[bA]])
                    tr.op("act", lambda e: e.activation(out=gbs[q4][:, :n], in_=ph(bA, 0, n), func=AF.Copy),
                          reads=[], writes=[gbB[q4], PB[bA]])
                    tr.op("dve", lambda e: e.tensor_tensor(out=u[:, j, 2:2 + n], in0=ph(bB, 0, n),
                                                           in1=gcs[s][:, :n], op=ALU.mult),
                          reads=[gcB[s]], writes=[uB[j], PB[bB]])
                    tr.op("act", lambda e: e.activation(out=c1[q4][:, :n], in_=u[:, j, 0:n], func=AF.Copy,
                                                        scale=cw[:, j, 0:1]),
                          reads=[uB[j], wB], writes=[c1B[q4]])

                def E2(ti, j):
                    t0, n = tiles[ti]
                    q4 = j % 4
                    tr.op("dve", lambda e: e.scalar_tensor_tensor(out=c2[q4][:, :n], in0=u[:, j, 1:1 + n],
                                                                  scalar=cw[:, j, 1:2], in1=c1[q4][:, :n],
                                                                  op0=ALU.mult, op1=ALU.add),
                          reads=[uB[j], wB, c1B[q4]], writes=[c2B[q4]])

                def E3(ti, j):
                    t0, n = tiles[ti]
                    q4 = j % 4
                    tr.op("dve", lambda e: e.scalar_tensor_tensor(out=c3[q4][:, :n], in0=u[:, j, 2:2 + n],
                                                                  scalar=cw[:, j, 2:3], in1=c2[q4][:, :n],
                                                                  op0=ALU.mult, op1=ALU.add),
                          reads=[uB[j], wB, c2B[q4]], writes=[c3B[q4]])

                def E4(ti, j):
                    t0, n = tiles[ti]
                    p2, q4 = ti % 2, j % 4
                    tr.op("dve", lambda e: e.tensor_tensor(out=y[p2][:, j, :n], in0=gbs[q4][:, :n],
                                                           in1=c3[q4][:, :n], op=ALU.mult),
                          reads=[c3B[q4], gbB[q4]], writes=[yB[p2]])
                    tr.op("act", lambda e: e.activation(out=u[:, j, 0:2], in_=u[:, j, n:n + 2], func=AF.Copy),
                          reads=[], writes=[uB[j]])

                def WOUT(ti):
                    t0, n = tiles[ti]
                    p2 = ti % 2
                    hb = hB[tile_idx(t0)]
                    for jo in range(NCH):
                        bank = (7, 6)[jo % 2]
                        for c in range(NCH):
                            tr.op("pe", lambda e: e.matmul(ph(bank, 0, n), lhsT=w_out[:, c, jo * 128:(jo + 1) * 128],
                                                           rhs=y[p2][:, c, :n], start=(c == 0), stop=(c == NCH - 1)),
                                  reads=[wB2, yB[p2]], writes=[PB[bank]])
                        tr.op("dve", lambda e: e.tensor_tensor(out=h[:, jo, t0:t0 + n], in0=ph(bank, 0, n),
                                                               in1=h[:, jo, t0:t0 + n], op=ALU.add),
                              reads=[hb], writes=[hb, PB[bank]])

                for f_ in NORM(0):
                    f_()
                slot = {0: 0, 1: 1, 2: 2, 3: 3, 5: 4}
                seq = [(ti, j) for ti in range(len(tiles)) for j in range(NCH)]

                def lagged(k, fn, lag):
                    if 0 <= k - lag < len(seq):
                        fn(*seq[k - lag])

                for k, (ti, j) in enumerate(seq):
                    if j == 0:
                        nst = NORM(ti + 1) if ti + 1 < len(tiles) else None
                    J_pe(ti, j)
                    E1(ti, j)
                    lagged(k, E2, 1)
                    lagged(k, E3, 2)
                    lagged(k, E4, 3)
                    if nst is not None and j in slot:
                        nst[slot[j]]()
                    if j == NCH - 1 and ti >= 1:
                        WOUT(ti - 1)
                for k in range(len(seq), len(seq) + 3):
                    lagged(k, E2, 1)
                    lagged(k, E3, 2)
                    lagged(k, E4, 3)
                WOUT(len(tiles) - 1)
                tr.barrier()

        def phase_moe(l, tiles, gi, final_gi=None):
            with ExitStack() as st_:
                xnb = sb(st_, "xnb", [128, NCH, TT], BF16)
                xnbB = Buf("xnb")
                wr = sb(st_, "wr", [128, NCH, 36], F32)
                rb = sb(st_, "rb", [128, 36], F32)
                rB = Buf("routerw")
                tr.dma("sp", "wr%d" % l, [
                    (wr[:], wr_d[l].rearrange("(c p) f -> p c f", p=128), (), [rB]),
                    (rb[:], rb_d[l], (), [rB]),
                ])
                wgu = [sb(st_, "wgu%d" % i, [128, 2, NCH, 256], BF16) for i in range(2)]
                wdn = [sb(st_, "wdn%d" % i, [128, 2, D], BF16) for i in range(2)]
                wB = [Buf("wexp%d" % i) for i in range(2)]

                def load_expert(e):
                    p = e % 2
                    tr.dma("pool", "wE%d" % p, [
                        (wgu[p][:].rearrange("p a c f -> p (a c f)"), wgu_d[l, e], (), [wB[p]]),
                        (wdn[p][:].rearrange("p a d -> p (a d)"), wdn_d[l, e], (), [wB[p]]),
                    ])
                load_expert(0)
                load_expert(1)
                ns = norm_scratch(st_)
                xn32_ = [sb(st_, "xn32_%d" % i, [128, NCH, TW], F32) for i in range(2)]
                xn32B_ = [Buf("xn32_%d" % i) for i in range(2)]
                NB = sum(n // 128 for (_, n) in tiles)
                R = {k: sb(st_, "r_" + k, [128, NB, w], F32) for k, w in
                     (("lg", 36), ("gmax", 1), ("gmask", 4), ("gsh", 4), ("gsum", 1), ("pen", 4),
                      ("em", 32), ("v1", 1), ("v2", 1), ("sel", 32), ("ex", 32), ("ssum", 1),
                      ("den", 1), ("comb", 32))}
                lgT = [sb(st_, "lgT%d" % i, [36, TW], F32) for i in range(2)]
                lgTB = [Buf("lgT%d" % i) for i in range(2)]
                ctile = [sb(st_, "ctile%d" % i, [128, 128], BF16) for i in range(2)]
                ctB = [Buf("ctile%d" % i) for i in range(2)]
                rtB = Buf("rtmp")
                psB = {"ss": 3}
                blk_cols = []
                for (t0, n) in tiles:
                    for b0 in range(0, n, 128):
                        blk_cols.append(t0 + b0)

                def NSTG(ti_):
                    t0, n = tiles[ti_]
                    x32 = xn32_[ti_ % 2]
                    return rmsnorm_stages(psB, t0, n, gi, lambda c: x32[:, c, :n], xn32B_[ti_ % 2], ns)

                def ROUTER(ti_):
                    t0, n = tiles[ti_]
                    xn32, xn32B = xn32_[ti_ % 2], xn32B_[ti_ % 2]
                    tr.op("act", lambda e: e.activation(out=xnb[:, :, t0:t0 + n], in_=xn32[:, :, :n], func=AF.Copy),
                          reads=[xn32B], writes=[xnbB])
                    lt, ltB = lgT[ti_ % 2], lgTB[ti_ % 2]
                    for c in range(NCH):
                        tr.op("pe", lambda e: e.matmul(psum[2][0:36, 0:n], lhsT=wr[:, c, :], rhs=xn32[:, c, :n],
                                                       start=(c == 0), stop=(c == NCH - 1)),
                              reads=[xn32B, rB], writes=[PB[2]])
                    tr.op("act", lambda e: e.activation(out=lt[:, :n], in_=psum[2][0:36, 0:n], func=AF.Copy),
                          reads=[], writes=[ltB, PB[2]])
                    for b0 in range(0, n, 128):
                        blk = blk_cols.index(t0 + b0)
                        bank = (6, 7)[blk % 2]
                        tr.op("pe", lambda e: e.transpose(out=psum[bank][:, 0:36], in_=lt[:, b0:b0 + 128],
                                                          identity=ident[0:36, 0:36]),
                              reads=[ltB, cB], writes=[PB[bank]])
                        tr.op("dve", lambda e: e.tensor_tensor(out=R["lg"][:, blk, :], in0=psum[bank][:, 0:36], in1=rb[:],
                                                               op=ALU.add),
                              reads=[rB], writes=[rtB, PB[bank]])

                for f_ in NSTG(0):
                    f_()
                for ti_ in range(len(tiles)):
                    nst = NSTG(ti_ + 1) if ti_ + 1 < len(tiles) else None
                    if nst is not None:
                        for f_ in nst[0:4]:
                            f_()
                    ROUTER(ti_)
                    if nst is not None:
                        nst[4]()
                V = lambda fn: tr.op("dve", fn, reads=[rtB], writes=[rtB])
                A = lambda fn: tr.op("act", fn, reads=[rtB], writes=[rtB])
                bc = lambda ap, w: ap.to_broadcast([128, NB, w])
                gl = R["lg"][:, :, 0:4]
                el = R["lg"][:, :, 4:36]
                V(lambda e: e.tensor_reduce(out=R["gmax"][:], in_=gl, axis=AX.X, op=ALU.max))
                V(lambda e: e.tensor_tensor(out=R["gmask"][:], in0=gl, in1=bc(R["gmax"][:], 4), op=ALU.is_ge))
                V(lambda e: e.tensor_tensor(out=R["gsh"][:], in0=gl, in1=bc(R["gmax"][:], 4), op=ALU.subtract))
                A(lambda e: e.activation(out=R["gsh"][:], in_=R["gsh"][:], func=AF.Exp))
                V(lambda e: e.tensor_reduce(out=R["gsum"][:], in_=R["gsh"][:], axis=AX.X, op=ALU.add))
                V(lambda e: e.tensor_scalar(out=R["pen"][:], in0=R["gmask"][:], scalar1=-1.0, scalar2=1e30,
                                            op0=ALU.add, op1=ALU.mult))
                V(lambda e: e.tensor_tensor(out=R["em"][:].rearrange("p b (g x) -> p b g x", g=4),
                                            in0=el.rearrange("p b (g x) -> p b g x", g=4),
                                            in1=R["pen"][:].unsqueeze(3).to_broadcast([128, NB, 4, 8]), op=ALU.add))
                V(lambda e: e.tensor_reduce(out=R["v1"][:], in_=R["em"][:], axis=AX.X, op=ALU.max))
                V(lambda e: e.tensor_tensor(out=R["sel"][:], in0=R["em"][:], in1=bc(R["v1"][:], 32), op=ALU.is_ge))
                V(lambda e: e.scalar_tensor_tensor(out=R["ex"][:], in0=R["sel"][:], scalar=-1e30, in1=R["em"][:],
                                                   op0=ALU.mult, op1=ALU.add))
                V(lambda e: e.tensor_reduce(out=R["v2"][:], in_=R["ex"][:], axis=AX.X, op=ALU.max))
                V(lambda e: e.tensor_tensor(out=R["sel"][:], in0=R["em"][:], in1=bc(R["v2"][:], 32), op=ALU.is_ge))
                V(lambda e: e.tensor_tensor(out=R["ex"][:], in0=R["em"][:], in1=bc(R["v1"][:], 32), op=ALU.subtract))
                A(lambda e: e.activation(out=R["ex"][:], in_=R["ex"][:], func=AF.Exp))
                V(lambda e: e.tensor_tensor(out=R["ex"][:], in0=R["ex"][:], in1=R["sel"][:], op=ALU.mult))
                V(lambda e: e.tensor_reduce(out=R["ssum"][:], in_=R["ex"][:], axis=AX.X, op=ALU.add))
                V(lambda e: e.tensor_tensor(out=R["den"][:], in0=R["gsum"][:], in1=R["ssum"][:], op=ALU.mult))
                V(lambda e: e.reciprocal(out=R["den"][:], in_=R["den"][:]))
                V(lambda e: e.tensor_tensor(out=R["comb"][:], in0=R["ex"][:], in1=bc(R["den"][:], 32), op=ALU.mult))
                c0 = tiles[0][0]
                scrB = [Buf("scr0"), Buf("scr1")]
                for gi_, b_ in enumerate(range(0, NB, 4)):
                    nb_ = min(4, NB - b_)
                    bank = (4, 5)[gi_ % 2]
                    ct = ctile[gi_ % 2]
                    tr.op("pe", lambda e: e.transpose(out=psum[bank][0:32 * nb_, 0:128],
                                                      in_=R["comb"][:, b_:b_ + nb_, :].rearrange("p b x -> p (b x)"),
                                                      identity=ident[:]),
                          reads=[rtB, cB], writes=[PB[bank]])
                    tr.op("act", lambda e: e.activation(out=ct[0:32 * nb_, :], in_=psum[bank][0:32 * nb_, 0:128], func=AF.Copy),
                          reads=[], writes=[ctB[gi_ % 2], PB[bank]])
                    tr.dma("sp", "scr%d_%d" % (l, gi_ % 2),
                           [(scr[l][:, blk_cols[b_ + k]:blk_cols[b_ + k] + 128], ct[32 * k:32 * k + 32, :], [ctB[gi_ % 2]], [scrB[gi_ % 2]])
                            for k in range(nb_)])
                cbc = [sb(st_, "cbc%d" % i, [128, TT], BF16) for i in range(2)]
                cbcB = [Buf("cbc%d" % i) for i in range(2)]
                sbuf_ = [sb(st_, "sil%d" % i, [128, 2, TW], BF16) for i in range(2)]
                tbuf = [sb(st_, "tb%d" % i, [128, 2, TW], BF16) for i in range(2)]
                hdn = [sb(st_, "hdn%d" % i, [128, 2, TW], BF16) for i in range(2)]
                sB = [Buf("sil%d" % i) for i in range(2)]
                tB_ = [Buf("tb%d" % i) for i in range(2)]
                hdB = [Buf("hdn%d" % i) for i in range(2)]
                gu_banks = [(0, 1), (2, 3)]

                def gu_ap(set_, q, n):
                    bank = gu_banks[set_][q // 2]
                    return ph(bank, q % 2, n)

                def gu_bufs(set_):
                    b0_, b1_ = gu_banks[set_]
                    return [PB[b0_], PB[b0_], PB[b1_], PB[b1_]]

                def gu_view(set_, which, n):
                    bank = gu_banks[set_][which]
                    return psum[bank][:, :].rearrange("p (a b) -> p a b", a=2)[:, :, :n]

                def load_cbc(e):
                    p = e % 2
                    tr.dma("sp", "cbc%d" % p, [(cbc[p][:, c0:TT], scr[l][e:e + 1, c0:TT].partition_broadcast(128),
                                               scrB, [cbcB[p]])])

                if final_gi is None:
                    steps = [(e, t0, n) for e in range(NE) for (t0, n) in tiles]
                else:
                    tail = []
                    for k in range(len(tiles) + 2):
                        if k < len(tiles):
                            tail.append((NE - 2,) + tiles[k])
                        if k >= 2:
                            tail.append((NE - 1,) + tiles[k - 2])
                    steps = [(e, t0, n) for e in range(NE - 2) for (t0, n) in tiles] + tail

                def GU(si):
                    e, t0, n = steps[si]
                    set_, p = si % 2, e % 2
                    for q in range(4):
                        for c in range(NCH):
                            tr.op("pe", lambda en: en.matmul(gu_ap(set_, q, n),
                                                             lhsT=wgu[p][:, q // 2, c, (q % 2) * 128:(q % 2 + 1) * 128],
                                                             rhs=xnb[:, c, t0:t0 + n], start=(c == 0), stop=(c == NCH - 1)),
                                  reads=[wB[p], xnbB], writes=[gu_bufs(set_)[q]])
                    gb = gu_bufs(set_)
                    tr.op("act", lambda en: en.activation(out=sbuf_[set_][:, :, :n], in_=gu_view(set_, 0, n), func=AF.Silu),
                          reads=[], writes=[sB[set_], gb[0]])
                    for f in range(2):
                        tr.op("dve", lambda en: en.tensor_tensor(out=tbuf[set_][:, f, :n], in0=sbuf_[set_][:, f, :n],
                                                                 in1=cbc[p][:, t0:t0 + n], op=ALU.mult),
                              reads=[sB[set_], cbcB[p]], writes=[tB_[set_]])
                    tr.op("dve", lambda en: en.tensor_tensor(out=hdn[set_][:, :, :n], in0=gu_view(set_, 1, n),
                                                             in1=tbuf[set_][:, :, :n], op=ALU.mult),
                          reads=[tB_[set_]], writes=[hdB[set_], gb[2]])


                def DN(si):
                    e, t0, n = steps[si]
                    set_, p = si % 2, e % 2
                    for jo in range(NCH):
                        bank, half = 4 + jo // 2, jo % 2
                        for f in range(2):
                            tr.op("pe", lambda en: en.matmul(ph(bank, half, n), lhsT=wdn[p][:, f, jo * 128:(jo + 1) * 128],
                                                             rhs=hdn[set_][:, f, :n], start=(f == 0), stop=(f == 1)),
                                  reads=[wB[p], hdB[set_]], writes=[PB[bank]])
                    hb = hB[tile_idx(t0)]
                    for b in range(4):
                        tr.op("dve", lambda en: en.tensor_tensor(
                            out=h[:, 2 * b:2 * b + 2, t0:t0 + n],
                            in0=psum[4 + b][:, :].rearrange("p (a b) -> p a b", a=2)[:, :, :n],
                            in1=h[:, 2 * b:2 * b + 2, t0:t0 + n], op=ALU.add),
                            reads=[hb], writes=[hb, PB[4 + b]])

                nt = len(tiles)
                if final_gi is not None:
                    fns = norm_scratch(st_)
                    fob = [sb(st_, "ob%d" % i, [128, NCH, TW], F32) for i in range(2)]
                    fobB = [Buf("ob%d" % i) for i in range(2)]
                    o3 = outT.rearrange("(c p) t -> p c t", p=128)

                    def FINAL(ti):
                        t0, n = tiles[ti]
                        p2 = ti % 2
                        rmsnorm({"ss": 3}, t0, n, final_gi, lambda c: fob[p2][:, c, :n], fobB[p2], fns)
                        tr.dma("sp", "out%d" % p2, [(o3[:, :, t0 - HALO:t0 - HALO + n], fob[p2][:, :, :n], [fobB[p2]], ())])
                load_cbc(0)
                GU(0)
                n_major = NE if final_gi is None else NE - 2
                tile_of = {t0: i for i, (t0, _) in enumerate(tiles)}
                for si in range(len(steps)):
                    e, t0_, _n = steps[si]
                    if si < n_major * nt and si % nt == 0:
                        if e + 1 < NE:
                            load_cbc(e + 1)
                        if e >= 1 and e + 1 < NE:
                            load_expert(e + 1)
                    if final_gi is not None and si == n_major * nt:
                        load_cbc(NE - 1)
                        load_expert(NE - 1)
                    if si + 1 < len(steps):
                        GU(si + 1)
                    DN(si)
                    if final_gi is not None and e == NE - 1 and tile_of[t0_] >= 1:
                        FINAL(tile_of[t0_] - 1)
                if final_gi is not None:
                    FINAL(nt - 1)
                    tr.final_wait(["out0", "out1"])
                tr.barrier()

        def phase_attn(stop_after_kv=False):
            with ExitStack() as st_:
                KT = sb(st_, "KT", [128, 4, NKV], BF16)
                Vx = sb(st_, "Vx", [128, NKB, 4, 128], BF16)
                cosb = sb(st_, "cosb", [128, NKV], BF16)
                sinb = sb(st_, "sinb", [128, NKV], BF16)
                kvalid = sb(st_, "kvalid", [128, NKB], F32)
                esrow = sb(st_, "esrow", [1, 2048], BF16)
                vsink = sb(st_, "vsink", [1, 128], BF16)
                mask = sb(st_, "mask", [128, 512], BF16)
                identb = sb(st_, "identb", [128, 128], BF16)
                ones4 = sb(st_, "ones4", [128, 4, 64], BF16)
                tabB = Buf("tables")
                KB_ = [Buf("K%d" % i) for i in range(NKB)]
                VB_ = [Buf("V%d" % i) for i in range(NKB)]
                tr.dma("pool", "tab", [
                    (cosb[:], cos_d[:], (), [tabB]),
                    (sinb[:], sin_d[:], (), [tabB]),
                    (vsink[:], vsink_d[:], (), [tabB]),
                    (mask[:], mask_d[:], (), [tabB]),
                    (identb[:], ident_d[:], (), [tabB]),
                ])
                wq = sb(st_, "wq", [128, NCH, D], BF16)
                wqp = sb(st_, "wqp", [128, NCH, D], BF16)
                wo = sb(st_, "wo", [128, NCH, D], BF16)
                wqB = Buf("wqo")
                woB = Buf("wo")
                ns = norm_scratch(st_)
                xn = [sb(st_, "xnC%d" % i, [128, NCH, TW], BF16) for i in range(2)]
                xnB = [Buf("xnC%d" % i) for i in range(2)]
                t1 = [sb(st_, "t1_%d" % i, [128, TW], F32) for i in range(2)]
                t2 = [sb(st_, "t2_%d" % i, [128, TW], F32) for i in range(2)]
                t1B = [Buf("t1_%d" % i) for i in range(2)]
                t2B = [Buf("t2_%d" % i) for i in range(2)]
                psC = {"ss": 5}

                def rope_proj(w_a, w_b, wBuf, col, xn_ap, xB, n, tcol, out_ap, outB, s):
                    for (w_, bank) in ((w_a, 2 * s), (w_b, 2 * s + 1)):
                        for c in range(NCH):
                            tr.op("pe", lambda e: e.matmul(ph(bank, 0, n), lhsT=w_[:, c, col:col + 128], rhs=xn_ap(c),
                                                           start=(c == 0), stop=(c == NCH - 1)),
                                  reads=[wBuf, xB], writes=[PB[bank]])
                    tr.op("dve", lambda e: e.tensor_tensor(out=t1[s][:, :n], in0=ph(2 * s, 0, n), in1=cosb[:, tcol:tcol + n],
                                                           op=ALU.mult), reads=[tabB], writes=[t1B[s], PB[2 * s]])
                    tr.op("dve", lambda e: e.tensor_tensor(out=t2[s][:, :n], in0=ph(2 * s + 1, 0, n), in1=sinb[:, tcol:tcol + n],
                                                           op=ALU.mult), reads=[tabB], writes=[t2B[s], PB[2 * s + 1]])
                    return lambda: tr.op("dve", lambda e: e.tensor_tensor(out=out_ap, in0=t1[s][:, :n], in1=t2[s][:, :n],
                                                                          op=ALU.add),
                                         reads=[t1B[s], t2B[s]], writes=outB)

                with ExitStack() as s1:
                    wkd = sb(s1, "wkd", [128, NCH, 512], BF16)
                    wkpd = sb(s1, "wkpd", [128, NCH, 512], BF16)
                    wv = sb(s1, "wv", [128, NCH, 256], BF16)
                    wB = Buf("wkv")
                    tr.dma("pool", "wC1", wload(wkd, wkd_d, wB) + wload(wkpd, wkpd_d, wB) + wload(wv, wv_d, wB))
                    tr.dma("pool", "wC2", wload(wq, wq_d, wqB) + wload(wqp, wqp_d, wqB))
                    tr.dma("pool", "wC2o", wload(wo, wo_d, woB))
                    with ExitStack() as s0:
                        esrow32 = sb(s0, "esrow32", [1, 1024], F32)
                        e32B = Buf("esrow32")
                        tr.dma("sp", "tab2", [(kvalid[:], kbias_d[:], (), [tabB])])
                        for hf in range(2):
                            tr.dma("sp", "tab3", [(esrow32[:], sinkrow_d[:, hf * 1024:(hf + 1) * 1024], (), [e32B])])
                            tr.op("act", lambda e: e.activation(out=esrow[:, hf * 1024:(hf + 1) * 1024], in_=esrow32[:],
                                                                func=AF.Exp), reads=[e32B], writes=[tabB])
                    tr.op("dve", lambda e: e.memset(ones4[:], 1.0), writes=[tabB])
                    for kb in range(NKB):
                        tr.op("act", lambda e: e.activation(out=Vx[:, kb, :, 64:128], in_=ones4[:], func=AF.Copy,
                                                            scale=kvalid[:, kb:kb + 1]),
                              reads=[tabB], writes=[VB_[kb]])
                    tiles = L0_MOE_TILES

                    def NORM1(ti):
                        t0, n = tiles[ti]
                        return rmsnorm_stages(psC, t0, n, 2, lambda c: xn[ti % 2][:, c, :n], xnB[ti % 2], ns)

                    for f_ in NORM1(0):
                        f_()
                    for ti, (t0, n) in enumerate(tiles):
                        p2 = ti % 2
                        kbs = [KB_[(t0 - 2) // 128 + i] for i in range(n // 128)]
                        nst = NORM1(ti + 1) if ti + 1 < len(tiles) else None
                        pend = None
                        for g in range(4):
                            fin = rope_proj(wkd, wkpd, wB, g * 128, lambda c: xn[p2][:, c, :n], xnB[p2], n, t0 - 2,
                                            KT[:, g, t0 - 2:t0 - 2 + n], kbs, g % 2)
                            if pend is not None:
                                pend()
                            pend = fin
                            if nst is not None:
                                if g == 0:
                                    nst[0](); nst[1]()
                                elif g == 1:
                                    nst[2](); nst[3]()
                                elif g == 2:
                                    nst[4]()
                        pend()
                        for b0 in range(0, n, 128):
                            kb = (t0 + b0 - 2) // 128
                            bank = 6 + kb % 2
                            for c in range(NCH):
                                tr.op("pe", lambda e: e.matmul(ph(bank, 0, 256), lhsT=xn[p2][:, c, b0:b0 + 128], rhs=wv[:, c, :],
                                                               start=(c == 0), stop=(c == NCH - 1)),
                                      reads=[wB, xnB[p2]], writes=[PB[bank]])
                            tr.op("act", lambda e: e.activation(out=Vx[:, kb, :, 0:64],
                                                                in_=ph(bank, 0, 256).rearrange("p (g d) -> p g d", g=4),
                                                                func=AF.Copy, scale=kvalid[:, kb:kb + 1]),
                                  reads=[tabB], writes=[VB_[kb], PB[bank]])
                    tr.barrier()
                if stop_after_kv:
                    return
                with ExitStack() as s2:
                    wB = wqB
                    wB2 = woB
                    QT = [sb(s2, "QT%d" % i, [128, NCH, TW], BF16) for i in range(2)]
                    QB = [Buf("QT%d" % i) for i in range(2)]
                    OT = [sb(s2, "OT%d" % i, [128, NCH, TW], BF16) for i in range(2)]
                    OB = [Buf("OT%d" % i) for i in range(2)]
                    PT = [sb(s2, "PT%d" % i, [128, 1024], BF16) for i in range(2)]
                    PTB = [Buf("PT%d" % i) for i in range(2)]
                    rden = [sb(s2, "rdenA%d" % i, [64, 512], F32) for i in range(2)]
                    rdB = [Buf("rdenA%d" % i) for i in range(2)]
                    s_banks = [(0, 1), (2, 3)]
                    o_banks = [6, 7]
                    tiles = L1_TILES

                    def QNORM(ti):
                        t0, n = tiles[ti]
                        p2 = ti % 2
                        return rmsnorm_stages(psC, t0, n, 3, lambda c: xn[p2][:, c, :n], xnB[p2], ns)

                    def QPROJ(ti):
                        t0, n = tiles[ti]
                        p2 = ti % 2
                        pend = None
                        for c8 in range(NCH):
                            fin = rope_proj(wq, wqp, wB, c8 * 128, lambda c: xn[p2][:, c, :n], xnB[p2], n, t0 - 2,
                                            QT[p2][:, c8, :n], [QB[p2]], c8 % 2)
                            if pend is not None:
                                pend()
                            pend = fin
                        pend()

                    def S_stage(ti, ui):
                        t0, n = tiles[ti]
                        p2 = ti % 2
                        qbl, g = ui // 4, ui % 4
                        sset = ui % 2
                        kb_cur = (t0 + 128 * qbl - 2) // 128
                        for kbi, kb in enumerate((kb_cur - 1, kb_cur)):
                            for half in range(2):
                                lo = 64 * half
                                bank = s_banks[sset][half]
                                tr.op("pe", lambda e: e.matmul(
                                    psum[bank][:, kbi * 256:(kbi + 1) * 256].rearrange("p (a b) -> p a b", a=2),
                                    lhsT=KT[lo:lo + 64, g, kb * 128:(kb + 1) * 128],
                                    rhs=QT[p2][lo:lo + 64, 2 * g:2 * g + 2, 128 * qbl:128 * qbl + 128],
                                    start=True, stop=False),
                                    reads=[KB_[kb], QB[p2]], writes=[PB[bank]])
                            for half in range(2):
                                bank = s_banks[sset][half]
                                tr.op("pe", lambda e: e.matmul(
                                    psum[bank][:, kbi * 256:(kbi + 1) * 256], lhsT=identb[:],
                                    rhs=mask[:, kbi * 256:(kbi + 1) * 256], start=False, stop=True),
                                    reads=[tabB], writes=[PB[bank]])
                        for half in range(2):
                            bank = s_banks[sset][half]
                            tr.op("act", lambda e: e.activation(
                                out=PT[sset][:, :].rearrange("p (k h x) -> p k h x", k=2, h=2)[:, :, half, :],
                                in_=psum[bank][:, :].rearrange("p (k x) -> p k x", k=2),
                                func=AF.Exp, scale=0.125),
                                reads=[], writes=[PTB[sset], PB[bank]])

                    def PV_stage(ti, ui):
                        t0, n = tiles[ti]
                        p2 = ti % 2
                        qbl, g = ui // 4, ui % 4
                        sset = ui % 2
                        kb_cur = (t0 + 128 * qbl - 2) // 128
                        ob_ = o_banks[ui % 2]
                        ob = [PB[ob_]]
                        rd, rB_ = rden[ui % 2], rdB[ui % 2]
                        tr.op("pe", lambda e: e.matmul(psum[ob_][:, :], lhsT=Vx[:, kb_cur - 1, g, :], rhs=PT[sset][:, 0:512],
                                                       start=True, stop=False),
                              reads=[VB_[kb_cur - 1], PTB[sset]], writes=ob)
                        tr.op("pe", lambda e: e.matmul(psum[ob_][:, :], lhsT=Vx[:, kb_cur, g, :], rhs=PT[sset][:, 512:1024],
                                                       start=False, stop=False),
                              reads=[VB_[kb_cur], PTB[sset]], writes=ob)
                        tr.op("pe", lambda e: e.matmul(psum[ob_][:, :], lhsT=vsink[0:1, :], rhs=esrow[0:1, g * 512:(g + 1) * 512],
                                                       start=False, stop=True),
                              reads=[tabB], writes=ob)
                        tr.op("act", lambda e: e.activation(out=rd[:, :], in_=psum[ob_][64:128, :], func=AF.Ln),
                              reads=[], writes=[rB_] + ob)

                    def PV_fin(ti, ui):
                        t0, n = tiles[ti]
                        p2 = ti % 2
                        qbl, g = ui // 4, ui % 4
                        ob_ = o_banks[ui % 2]
                        ob = [PB[ob_]]
                        rd, rB_ = rden[ui % 2], rdB[ui % 2]
                        tr.op("act", lambda e: e.activation(out=rd[:, :], in_=rd[:, :], func=AF.Exp, scale=-1.0),
                              reads=[], writes=[rB_])
                        for half in range(2):
                            lo = 64 * half
                            tr.op("dve", lambda e: e.tensor_tensor(
                                out=OT[p2][lo:lo + 64, 2 * g:2 * g + 2, 128 * qbl:128 * qbl + 128],
                                in0=psum[ob_][0:64, half * 256:(half + 1) * 256].rearrange("p (a b) -> p a b", a=2),
                                in1=rd[:, half * 256:(half + 1) * 256].rearrange("p (a b) -> p a b", a=2),
                                op=ALU.mult),
                                reads=[rB_], writes=[OB[p2]] + ob)

                    def OPROJ(ti, jos=range(NCH), one_bank=False):
                        t0, n = tiles[ti]
                        p2 = ti % 2
                        hb = hB[tile_idx(t0)]
                        for jo in jos:
                            bank = 4 if one_bank else 4 + jo % 2
                            for c in range(NCH):
                                tr.op("pe", lambda e: e.matmul(ph(bank, 0, n), lhsT=wo[:, c, jo * 128:(jo + 1) * 128],
                                                               rhs=OT[p2][:, c, :n], start=(c == 0), stop=(c == NCH - 1)),
                                      reads=[wB2, OB[p2]], writes=[PB[bank]])
                            tr.op("dve", lambda e: e.tensor_tensor(out=h[:, jo, t0:t0 + n], in0=ph(bank, 0, n),
                                                                   in1=h[:, jo, t0:t0 + n], op=ALU.add),
                                  reads=[hb], writes=[hb, PB[bank]])

                    for f_ in QNORM(0):
                        f_()
                    QPROJ(0)
                    slot = {0: 0, 1: 1, 2: 2, 3: 3, 5: 4}
                    for ti in range(len(tiles)):
                        nu = (tiles[ti][1] // 128) * 4
                        nst = QNORM(ti + 1) if ti + 1 < len(tiles) else None
                        S_stage(ti, 0)
                        for ui in range(nu):
                            if ui + 1 < nu:
                                S_stage(ti, ui + 1)
                            PV_stage(ti, ui)
                            if ui >= 1:
                                PV_fin(ti, ui - 1)
                            if nst is not None and ui in slot:
                                nst[slot[ui]]()
                            if ti >= 1:
                                OPROJ(ti - 1, jos=[ui], one_bank=True)
                        PV_fin(ti, nu - 1)
                        if ti + 1 < len(tiles):
                            QPROJ(ti + 1)
                    OPROJ(len(tiles) - 1)
                    tr.barrier()

        def phase_final():
            with ExitStack() as st_:
                ns = norm_scratch(st_)
                ob = [sb(st_, "ob%d" % i, [128, NCH, TW], F32) for i in range(2)]
                obB = [Buf("ob%d" % i) for i in range(2)]
                psE = {"ss": 3}
                o3 = outT.rearrange("(c p) t -> p c t", p=128)
                for ti, (t0, n) in enumerate(L1_TILES):
                    p2 = ti % 2
                    rmsnorm(psE, t0, n, 5, lambda c: ob[p2][:, c, :n], obB[p2], ns)
                    tr.dma("sp", "out%d" % p2, [(o3[:, :, t0 - HALO:t0 - HALO + n], ob[p2][:, :, :n], [obB[p2]], ())])
                tr.final_wait(["out0", "out1"])

        stages = [("init", lambda: None), ("conv", phase_conv), ("moe0", lambda: phase_moe(0, L0_MOE_TILES, 1)), ("attn", phase_attn),
                  ("moe1", lambda: phase_moe(1, L1_TILES, 4, final_gi=(None if debug else 5)))]
        if debug == "attn_kv":
            stages = [("conv", phase_conv), ("attn_kv", lambda: phase_attn(True))]
        if debug == "attn_only":
            stages = [("conv", phase_conv), ("attn_only", phase_attn)]
        done = False
        for name, fn in stages:
            fn()
            if debug == name:
                dump_h()
                done = True
                break
        if not done:
            if debug:
                phase_final()
                dump_h()
        else:
            with ExitStack() as st_:
                z = sb(st_, "zz", [128, NCH, TW], F32)
                zB = Buf("zz")
                tr.op("dve", lambda e: e.memset(z[:], 0.0), writes=[zB])
                o3 = outT.rearrange("(c p) t -> p c t", p=128)
                tr.dma("sp", "outz", [(o3[:, :, k * TW:(k + 1) * TW], z[:], [zB], ()) for k in range(OWN // TW)])
                tr.final_wait(["outz"])
    return nc


def _q_cols():
    cols, cols_p = [], []
    perm = (np.arange(64) + 32) % 64
    for c8 in range(8):
        g, i = c8 // 2, c8 % 2
        for hd in (4 * g + i, 4 * g + 2 + i):
            cols.append(hd * 64 + np.arange(64))
            cols_p.append(hd * 64 + perm)
    return np.concatenate(cols), np.concatenate(cols_p)


def prep_shared(inp):
    f = lambda a: np.ascontiguousarray(a, dtype=np.float32)
    S = {}
    g6 = np.stack([inp["conv_norm_g"][0], inp["ffn_norm_g"][0], inp["kv_norm_g"], inp["attn_norm_g"][0],
                   inp["ffn_norm_g"][1], inp["final_norm_g"]])
    S["gains"] = f(g6.reshape(6, 8, 128).transpose(2, 0, 1))
    S["ident"] = np.eye(128, dtype=np.float32)
    S["w_in"] = f(inp["conv_w_in"][0])
    S["cw"] = f(inp["conv_w"][0].reshape(3, 8, 128).transpose(2, 1, 0))
    S["w_out"] = f(inp["conv_w_out"][0])
    wk, wv = inp["w_kv"][:, :256], inp["w_kv"][:, 256:]
    perm = (np.arange(64) + 32) % 64
    kc = np.concatenate([np.concatenate([g * 64 + np.arange(64)] * 2) for g in range(4)])
    kcp = np.concatenate([np.concatenate([g * 64 + perm] * 2) for g in range(4)])
    S["wkd"] = f(wk[:, kc])
    S["wkpd"] = f(wk[:, kcp])
    S["wv"] = f(wv)
    qc, qcp = _q_cols()
    S["wq"] = f(inp["w_q"][0][:, qc])
    S["wqp"] = f(inp["w_q"][0][:, qcp])
    S["wo"] = f(inp["w_o"][0][qc, :])
    sk = inp["sinks"][0]
    row = np.zeros((4, 4, 128), np.float32)
    for g in range(4):
        for hh in range(4):
            half, i = hh // 2, hh % 2
            row[g, hh, :] = sk[4 * g + i + 2 * half]
    S["sinkrow"] = row.reshape(1, 2048)
    vs = np.zeros((1, 128), np.float32)
    vs[0, 64:] = 1.0
    S["vsink"] = vs
    k = np.arange(128)[:, None]
    q = np.arange(128)[None, :]
    m = np.zeros((128, 2, 2, 128), np.float32)
    m[:, 0] = np.where(k > q, 0.0, -30000.0).astype(np.float32)[:, None, :]
    m[:, 1] = np.where(k <= q, 0.0, -30000.0).astype(np.float32)[:, None, :]
    S["mask"] = m.reshape(128, 512)
    S["wr"] = f(np.concatenate([inp["router_group_w"], inp["router_expert_w"]], axis=2))
    rb = np.concatenate([inp["router_group_b"], inp["router_expert_b"]], axis=1)
    S["rb"] = f(np.broadcast_to(rb[:, None, :], (2, 128, 36)))
    wg = inp["w_gate"].reshape(2, NE, 8, 128, 256).transpose(0, 1, 3, 2, 4)
    wu = inp["w_up"].reshape(2, NE, 8, 128, 256).transpose(0, 1, 3, 2, 4)
    S["wgu"] = f(np.stack([wg, wu], axis=3).reshape(2, NE, 128, 2 * 8 * 256))
    S["wdn"] = f(inp["w_down"].reshape(2, NE, 2, 128, D).transpose(0, 1, 3, 2, 4).reshape(2, NE, 128, 2 * D))
    return S


def prep_core(inp, b, c):
    P0 = NMETA + c * OWN - HALO
    hfull = np.concatenate([inp["meta_tokens"].astype(np.float32), inp["x"][b]], axis=0)
    pos = P0 + np.arange(TT)
    rows = np.zeros((TT, D), np.float32)
    ok = pos >= 0
    rows[ok] = hfull[pos[ok]]
    C = {"xT": np.ascontiguousarray(rows.T)}
    half = 32
    inv = (10000.0 ** (-np.arange(half, dtype=np.float32) / half)).astype(np.float32)
    tp = pos[2:].astype(np.float32)
    ang = tp[None, :] * inv[np.arange(128) % 32][:, None]
    sign = np.where((np.arange(128) % 64) < 32, -1.0, 1.0).astype(np.float32)[:, None]
    C["cos"] = np.cos(ang).astype(np.float32)
    C["sin"] = (np.sin(ang) * sign).astype(np.float32)
    kpos = pos[2:].reshape(NKB, 128).T
    C["kbias"] = np.where(kpos >= 0, 1.0, 0.0).astype(np.float32)
    return C


_NC_CACHE = {}


def kernel(**inputs):
    inp = {k: np.asarray(v) for k, v in inputs.items()}
    S = prep_shared(inp)
    in_maps = []
    for core in range(8):
        b, c = core // 4, core % 4
        m = dict(S)
        m.update(prep_core(inp, b, c))
        in_maps.append(m)
    if "nc" not in _NC_CACHE:
        _NC_CACHE["nc"] = build_program()
    res = run_bass_kernel_spmd(_NC_CACHE["nc"], in_maps, core_ids=list(range(8)))
    out = np.empty((2, SEQ, D), np.float32)
    for core in range(8):
        b, c = core // 4, core % 4
        out[b, c * OWN:(c + 1) * OWN, :] = res.results[core]["outT"].T
    return out
```
